# Optimizing a Trainium2 kernel written in Bass

```python
import math
import jax
import jax.numpy as jnp
from jax import lax
import numpy as np

D_MODEL = 2048
BATCH = 8
SEQ = 2048
DEPTH = 1

BRANCH_WIDTH = D_MODEL // 2
N_BRANCHES = 2
HEAD_DIM = 128
ATTN_GROUPS = 3
WINDOWS = (128, 512, 2048)
DILATIONS = (1, 4, 16)
ATTN_HEADS = BRANCH_WIDTH // HEAD_DIM
QKV_WIDTH = ATTN_GROUPS * ATTN_HEADS * HEAD_DIM
ATTN_BLOCK = 128
NUM_BUCKETS = 32
MAX_DISTANCE = 2048
SGU_CHUNK = 128
SGU_GROUP_DIM = 128
SGU_GROUPS = BRANCH_WIDTH // SGU_GROUP_DIM
IN_COLS = 3 * QKV_WIDTH + 2 * BRANCH_WIDTH + N_BRANCHES * D_MODEL
N_EXPERTS = 32
TOP_K = 4
EXPERT_HIDDEN = D_MODEL
SWIGLU_ALPHA = 1.702
SWIGLU_LIMIT = 7.0
MOE_BLOCK = 512
LN_EPS = 1e-5

kernel_name = "hybrid_dilated_attn_gmlp_moe_deepnorm"


def layer_norm(x, g, b):
    xf = x.astype(jnp.float32)
    mu = jnp.mean(xf, axis=-1, keepdims=True)
    var = jnp.mean(jnp.square(xf - mu), axis=-1, keepdims=True)
    return ((xf - mu) * lax.rsqrt(var + LN_EPS) * g + b).astype(x.dtype)


def t5_bucket(dist):
    exact = NUM_BUCKETS // 2
    d = jnp.maximum(dist, 1).astype(jnp.float32)
    large = exact + (jnp.log(d / exact) / math.log(MAX_DISTANCE / exact)
                     * (NUM_BUCKETS - exact)).astype(jnp.int32)
    large = jnp.minimum(large, NUM_BUCKETS - 1)
    return jnp.where(dist < exact, dist, large)


def dilated_group_attention(q, k, v, bias_table, window, dilation):
    B, S, H, Dh = q.shape
    r = dilation
    span = window // dilation
    L = S // r
    nb = -(-L // ATTN_BLOCK)
    Lp = nb * ATTN_BLOCK

    def to_sub(t):
        t = t.reshape(B, L, r, H, Dh).transpose(0, 2, 1, 3, 4)
        t = jnp.pad(t, ((0, 0), (0, 0), (0, Lp - L), (0, 0), (0, 0)))
        return t.reshape(B, r, nb, ATTN_BLOCK, H, Dh)

    def with_prev(t):
        prev = jnp.pad(t[:, :, :-1], ((0, 0), (0, 0), (1, 0), (0, 0), (0, 0), (0, 0)))
        return jnp.concatenate([prev, t], axis=3)

    qb = to_sub(q)
    kw = with_prev(to_sub(k))
    vw = with_prev(to_sub(v))
    qi = jnp.arange(ATTN_BLOCK, dtype=jnp.int32)[:, None]
    kj = jnp.arange(2 * ATTN_BLOCK, dtype=jnp.int32)[None, :]
    sub_dist = qi + ATTN_BLOCK - kj
    band = (sub_dist >= 0) & (sub_dist <= span)
    bias = bias_table[t5_bucket(r * jnp.clip(sub_dist, 0, span))]
    bias = bias.transpose(2, 0, 1).astype(jnp.float32)
    has_prev = (jnp.arange(nb)[:, None, None] > 0) | (kj[None] >= ATTN_BLOCK)
    valid = band[None] & has_prev

    scores = jnp.einsum('brnqhd,brnkhd->brnhqk', qb, kw).astype(jnp.float32)
    scores = scores * (HEAD_DIM ** -0.5) + bias
    scores = jnp.where(valid[:, None], scores, -jnp.inf)
    m = jnp.max(scores, axis=-1, keepdims=True)
    p = jnp.exp(scores - m)
    denom = jnp.sum(p, axis=-1, keepdims=True)
    o = jnp.einsum('brnhqk,brnkhd->brnqhd', (p / denom).astype(v.dtype), vw)
    lse = (m + jnp.log(denom))[..., 0]
    o = o.reshape(B, r, Lp, H, Dh)[:, :, :L].transpose(0, 2, 1, 3, 4).reshape(B, S, H, Dh)
    lse = lse.transpose(0, 1, 2, 4, 3).reshape(B, r, Lp, H)[:, :, :L]
    lse = lse.transpose(0, 2, 1, 3).reshape(B, S, H)
    return o, lse


def dilated_attention(q, k, v, bias_table):
    outs, lses = [], []
    for g in range(ATTN_GROUPS):
        o, l = dilated_group_attention(q[:, :, g], k[:, :, g], v[:, :, g],
                                       bias_table[:, g], WINDOWS[g], DILATIONS[g])
        outs.append(o)
        lses.append(l)
    out = jnp.stack(outs, axis=0)
    lse = jnp.stack(lses, axis=0)
    w = jax.nn.softmax(lse, axis=0)
    return jnp.sum(w[..., None].astype(out.dtype) * out, axis=0)


def spatial_gating(z, ln_g, ln_b, w_s, b_s):
    B, S, _ = z.shape
    z = jax.nn.gelu(z)
    u, v = z[..., :BRANCH_WIDTH], z[..., BRANCH_WIDTH:]
    v = layer_norm(v, ln_g, ln_b)
    vc = v.reshape(B, S // SGU_CHUNK, SGU_CHUNK, SGU_GROUPS, SGU_GROUP_DIM)
    causal = jnp.tril(jnp.ones((SGU_CHUNK, SGU_CHUNK), dtype=bool))
    w = jnp.where(causal[None], w_s, 0.0).astype(v.dtype)
    mixed = jnp.einsum('gij,bcjgd->bcigd', w, vc) + b_s.T[None, None, :, :, None]
    return u * mixed.reshape(B, S, BRANCH_WIDTH)


def moe_ffn(h, w_router, b_router, w_gu, b_gu, w_down, b_down):
    B, S, D = h.shape
    T = B * S
    A = T * TOP_K
    ht = h.reshape(T, D)
    logits = (ht @ w_router + b_router).astype(jnp.float32)
    top_val, top_idx = lax.top_k(logits, TOP_K)
    gate = jax.nn.softmax(top_val, axis=-1)
    flat_e = top_idx.reshape(A).astype(jnp.int32)
    flat_tok = jnp.repeat(jnp.arange(T, dtype=jnp.int32), TOP_K)
    flat_gate = gate.reshape(A)
    order = jnp.argsort(flat_e)
    sorted_e = flat_e[order]
    counts = jnp.bincount(flat_e, length=N_EXPERTS).astype(jnp.int32)
    padded = (counts + MOE_BLOCK - 1) // MOE_BLOCK * MOE_BLOCK
    start = jnp.cumsum(counts) - counts
    pend = jnp.cumsum(padded)
    pstart = pend - padded
    rank = jnp.arange(A, dtype=jnp.int32) - start[sorted_e]
    dest = pstart[sorted_e] + rank
    n_blocks = (A + N_EXPERTS * MOE_BLOCK + MOE_BLOCK - 1) // MOE_BLOCK
    cap = n_blocks * MOE_BLOCK
    slot_tok = jnp.zeros((cap,), jnp.int32).at[dest].set(flat_tok[order])
    slot_gate = jnp.zeros((cap,), jnp.float32).at[dest].set(flat_gate[order])
    block_start = jnp.arange(n_blocks, dtype=jnp.int32) * MOE_BLOCK
    block_e = jnp.minimum(jnp.searchsorted(pend, block_start, side='right'), N_EXPERTS - 1)

    def expert_block(args):
        e, tok = args
        xb = ht[tok]
        hb = xb @ w_gu[e] + b_gu[e]
        glu = jnp.minimum(hb[:, :EXPERT_HIDDEN], SWIGLU_LIMIT)
        lin = jnp.clip(hb[:, EXPERT_HIDDEN:], -SWIGLU_LIMIT, SWIGLU_LIMIT)
        act = glu * jax.nn.sigmoid(SWIGLU_ALPHA * glu) * (lin + 1.0)
        return act @ w_down[e] + b_down[e]

    y = lax.map(expert_block, (block_e, slot_tok.reshape(n_blocks, MOE_BLOCK)))
    y = (y.reshape(cap, D) * slot_gate[:, None]).astype(h.dtype)
    out = jnp.zeros((T, D), h.dtype).at[slot_tok].add(y)
    return out.reshape(B, S, D)


def setup_inputs(seed: int = 0) -> dict:
    key = jax.random.key(seed)
    ks = jax.random.split(key, 24)
    f32 = jnp.float32
    L = DEPTH
    beta = (8.0 * DEPTH) ** -0.25

    def nrm(k, shape, scale):
        return jax.random.normal(k, shape, f32) * scale

    x = nrm(ks[0], (BATCH, SEQ, D_MODEL), 1.0)
    w_qk = nrm(ks[1], (L, D_MODEL, 2 * QKV_WIDTH), D_MODEL ** -0.5)
    w_v = nrm(ks[2], (L, D_MODEL, QKV_WIDTH), beta * D_MODEL ** -0.5)
    w_z = nrm(ks[3], (L, D_MODEL, 2 * BRANCH_WIDTH), D_MODEL ** -0.5)
    w_g = nrm(ks[4], (L, D_MODEL, N_BRANCHES * D_MODEL), D_MODEL ** -0.5)
    w_in = jnp.concatenate([w_qk, w_v, w_z, w_g], axis=-1)
    rel_bias = nrm(ks[5], (NUM_BUCKETS, ATTN_GROUPS * ATTN_HEADS), 0.5)
    sgu_ln_g = 1.0 + nrm(ks[6], (L, BRANCH_WIDTH), 0.05)
    sgu_ln_b = nrm(ks[7], (L, BRANCH_WIDTH), 0.02)
    sgu_w = nrm(ks[8], (L, SGU_GROUPS, SGU_CHUNK, SGU_CHUNK), SGU_CHUNK ** -0.5)
    sgu_b = 1.0 + nrm(ks[9], (L, SGU_GROUPS, SGU_CHUNK), 0.01)
    w_branch = nrm(ks[10], (L, N_BRANCHES, BRANCH_WIDTH, D_MODEL), beta * BRANCH_WIDTH ** -0.5)
    w_out = nrm(ks[11], (L, D_MODEL, D_MODEL), beta * D_MODEL ** -0.5)
    ln1_g = 1.0 + nrm(ks[12], (L, D_MODEL), 0.05)
    ln1_b = nrm(ks[13], (L, D_MODEL), 0.02)
    w_router = nrm(ks[14], (L, D_MODEL, N_EXPERTS), D_MODEL ** -0.5)
    b_router = nrm(ks[15], (L, N_EXPERTS), 0.01)
    w_gu = nrm(ks[16], (L, N_EXPERTS, D_MODEL, 2 * EXPERT_HIDDEN), beta * D_MODEL ** -0.5)
    b_gu = nrm(ks[17], (L, N_EXPERTS, 2 * EXPERT_HIDDEN), 0.01)
    w_down = nrm(ks[18], (L, N_EXPERTS, EXPERT_HIDDEN, D_MODEL), beta * EXPERT_HIDDEN ** -0.5)
    b_down = nrm(ks[19], (L, N_EXPERTS, D_MODEL), 0.01)
    ln2_g = 1.0 + nrm(ks[20], (L, D_MODEL), 0.05)
    ln2_b = nrm(ks[21], (L, D_MODEL), 0.02)
    return {"x": x, "w_in": w_in, "rel_bias": rel_bias, "sgu_ln_g": sgu_ln_g,
            "sgu_ln_b": sgu_ln_b, "sgu_w": sgu_w, "sgu_b": sgu_b, "w_branch": w_branch,
            "w_out": w_out, "ln1_g": ln1_g, "ln1_b": ln1_b, "w_router": w_router,
            "b_router": b_router, "w_gu": w_gu, "b_gu": b_gu, "w_down": w_down,
            "b_down": b_down, "ln2_g": ln2_g, "ln2_b": ln2_b}


def reference(x, w_in, rel_bias, sgu_ln_g, sgu_ln_b, sgu_w, sgu_b, w_branch, w_out,
              ln1_g, ln1_b, w_router, b_router, w_gu, b_gu, w_down, b_down, ln2_g, ln2_b):
    alpha = (2.0 * DEPTH) ** 0.25
    B, S, D = x.shape
    bias_table = rel_bias.reshape(NUM_BUCKETS, ATTN_GROUPS, ATTN_HEADS)
    o_k, o_v, o_z, o_g = QKV_WIDTH, 2 * QKV_WIDTH, 3 * QKV_WIDTH, 3 * QKV_WIDTH + 2 * BRANCH_WIDTH
    h = x
    for l in range(DEPTH):
        proj = h @ w_in[l]
        q = proj[..., :o_k].reshape(B, S, ATTN_GROUPS, ATTN_HEADS, HEAD_DIM)
        k = proj[..., o_k:o_v].reshape(B, S, ATTN_GROUPS, ATTN_HEADS, HEAD_DIM)
        v = proj[..., o_v:o_z].reshape(B, S, ATTN_GROUPS, ATTN_HEADS, HEAD_DIM)
        z = proj[..., o_z:o_g]
        gates = jax.nn.sigmoid(proj[..., o_g:].reshape(B, S, N_BRANCHES, D))
        y_attn = dilated_attention(q, k, v, bias_table).reshape(B, S, BRANCH_WIDTH)
        y_sgu = spatial_gating(z, sgu_ln_g[l], sgu_ln_b[l], sgu_w[l], sgu_b[l])
        branches = jnp.stack([y_attn, y_sgu], axis=2)
        y = jnp.einsum('bsnc,ncd->bsnd', branches, w_branch[l])
        mixed = jnp.sum(gates * y, axis=2) @ w_out[l]
        h = layer_norm(alpha * h + mixed, ln1_g[l], ln1_b[l])
        ffn = moe_ffn(h, w_router[l], b_router[l], w_gu[l], b_gu[l], w_down[l], b_down[l])
        h = layer_norm(alpha * h + ffn, ln2_g[l], ln2_b[l])
    return h
```

```python
import math
from contextlib import ExitStack
import numpy as np
import concourse.bass as bass
import concourse.mybir as mybir
from concourse.bass_utils import run_bass_kernel_spmd

dt = mybir.dt
F32, BF16, I32 = dt.float32, dt.bfloat16, dt.int32
AF = mybir.ActivationFunctionType
ALU = mybir.AluOpType

SAME_ENGINE_SYNC = True
DEBUG = False
NCORES = 8

D = 2048
S = 2048
NH = 8
NG = 3
DIL = (1, 4, 16)
NE = 32
CAP = 384
NPRE = 0
NST = CAP // 128
ALPHA = 2.0 ** 0.25
EPS = 1e-5
SCALE = 128.0 ** -0.5
NEG = -30000.0
O_K, O_V, O_Z, O_G = 3072, 6144, 9216, 11264
ARENA = 204 * 1024


class _Op:
    __slots__ = ("eng", "fn", "deps", "dma", "semkey", "need_inc", "count")

    def __init__(self, eng, fn, deps, dma, semkey):
        self.eng = eng
        self.fn = fn
        self.deps = deps
        self.dma = dma
        self.semkey = semkey
        self.need_inc = False
        self.count = 0


class Prog:
    def __init__(self, nc):
        self.nc = nc
        self.ops = []
        self.res = {}
        self.pending_barrier = None

    def add(self, eng, fn, reads=(), writes=(), dma=False, semkey=None, nowaw=False):
        idx = len(self.ops)
        deps = set()
        for r in reads:
            st = self.res.get(r)
            if st is not None:
                deps.update(st[0])
        for w in writes:
            st = self.res.get(w)
            if st is None:
                continue
            if st[1]:
                deps.update(st[1])
                deps.update(st[0])
            elif not nowaw:
                deps.update(st[0])
        for r in reads:
            st = self.res.setdefault(r, [[], []])
            st[1].append(idx)
        for w in writes:
            st = self.res.setdefault(w, [[], []])
            others = [r for r in st[1] if r != idx]
            if others:
                st[0] = [idx]
                st[1] = []
            else:
                st[1] = []
                if nowaw:
                    st[0].append(idx)
                else:
                    st[0] = [idx]
        deps.discard(idx)
        if dma and semkey is None:
            semkey = ("dma", writes[0] if writes else idx)
        self.ops.append(_Op(eng, fn, deps, dma, semkey))
        return idx

    def pe(self, fn, reads=(), writes=(), **kw):
        return self.add("pe", fn, reads, writes, **kw)

    def act(self, fn, reads=(), writes=(), **kw):
        return self.add("act", fn, reads, writes, **kw)

    def dve(self, fn, reads=(), writes=(), **kw):
        return self.add("dve", fn, reads, writes, **kw)

    def pool(self, fn, reads=(), writes=(), **kw):
        return self.add("pool", fn, reads, writes, **kw)

    def dma(self, q, fn, reads=(), writes=(), semkey=None, nowaw=True):
        return self.add(q, fn, reads, writes, dma=True, semkey=semkey, nowaw=nowaw)

    def barrier(self, engines=("sp", "pe", "act", "dve", "pool")):
        deps = set()
        for st in self.res.values():
            deps.update(st[0])
            deps.update(st[1])
        idx = len(self.ops)
        op = _Op("sp", lambda e: e.nop(), deps, False, None)
        self.ops.append(op)
        self.res = {"__bar__": [[idx], []]}
        for en in engines:
            if en == "sp":
                continue
            self.add(en, lambda e: e.nop(), reads=["__bar__"])

    def emit(self, stack):
        nc = self.nc
        ops = self.ops
        for i, op in enumerate(ops):
            for d in op.deps:
                p = ops[d]
                if p.dma:
                    p.need_inc = True
                elif p.eng == op.eng and (p.eng == "pe" or not SAME_ENGINE_SYNC):
                    pass
                else:
                    p.need_inc = True
        sems = {}

        def get_sem(key):
            if key not in sems:
                sems[key] = stack.enter_context(nc.semaphore("s%d" % len(sems)))
            return sems[key]

        counters = {}
        for op in ops:
            if op.dma:
                op.need_inc = True
                k = op.semkey
                counters[k] = counters.get(k, 0) + 16
                op.count = counters[k]
                get_sem(k)
            elif op.need_inc:
                k = ("eng", op.eng)
                counters[k] = counters.get(k, 0) + 1
                op.count = counters[k]
                op.semkey = k
                get_sem(k)
        self.n_sems = len(sems)
        by_eng = {}
        for i, op in enumerate(ops):
            by_eng.setdefault(op.eng, []).append(i)
        block = stack.enter_context(nc.Block())
        deco = {"pe": block.tensor, "act": block.scalar, "dve": block.vector,
                "pool": block.gpsimd, "sp": block.sync}

        def make(engname):
            idxs = by_eng.get(engname, [])

            def body(eng):
                seen = {}
                for i in idxs:
                    op = ops[i]
                    need = {}
                    for d in op.deps:
                        p = ops[d]
                        if (not p.dma) and p.eng == engname and (engname == "pe" or not SAME_ENGINE_SYNC):
                            continue
                        k = p.semkey
                        if need.get(k, 0) < p.count:
                            need[k] = p.count
                    for k, v in need.items():
                        if seen.get(k, 0) < v:
                            eng.wait_ge(sems[k], v)
                            seen[k] = v
                    ins = op.fn(eng)
                    if op.need_inc:
                        ins.then_inc(sems[op.semkey], 16 if op.dma else 1)
            return body

        for engname in ["sp", "pe", "act", "dve", "pool"]:
            if engname in by_eng:
                deco[engname](make(engname))


def _dsize(d):
    return {F32: 4, BF16: 2, I32: 4}[d]


def build_program():
    nc = bass.Bass("TRN2", target_bir_lowering=False)
    scr_kind = "ExternalOutput" if DEBUG else "Internal"

    def din(name, shape, d=F32):
        return nc.dram_tensor(name, list(shape), d, kind="ExternalInput").ap()

    xT_d = din("xT", [128, 16 * S])
    x_d = din("x", [S, D])
    wz_d = din("wz", [4, 128, 16 * 512])
    wa_d = din("wa", [24, 128, 16 * 384])
    wg_d = din("wg", [8, 128, 16 * 512])
    bm_d = din("bm", [24, 128, 256])
    swt_d = din("swt", [128, 8 * 128])
    tril_d = din("tril", [128, 128])
    sgub_d = din("sgub", [1, 8 * 128])
    slng_d = din("slng", [1, 1024])
    slnb_d = din("slnb", [1, 1024])
    wbr_d = din("wbr", [16, 128, 2 * 8 * 128])
    wout_d = din("wout", [128, 16 * D])
    ln1g_d = din("ln1g", [1, D])
    ln1b_d = din("ln1b", [1, D])
    wr_d = din("wr", [128, 16 * 32])
    br_d = din("br", [1, 32])
    wgu_d = din("wgu", [NE, 8, 128, 16 * 512])
    bgu_d = din("bgu", [128, NE * 32])
    wd_d = din("wd", [NE, 4, 128, 16 * 512])
    bd_d = din("bd", [NE, D])
    ln2g_d = din("ln2g", [1, D])
    ln2b_d = din("ln2b", [1, D])
    ident_d = din("ident", [128, 128])
    ustr_d = din("ustr", [128, 128])
    ecrow_d = din("ecrow", [1, 32])
    tokid_d = din("tokid", [128, 16])

    out_d = nc.dram_tensor("out", [S, D], F32, kind="ExternalOutput").ap()
    ysgu_d = nc.dram_tensor("ysgu_s", [128, 8 * S], BF16, kind=scr_kind).ap()
    yattn_d = nc.dram_tensor("yattn_s", [8, 128, S], BF16, kind=scr_kind).ap()
    sg_d = nc.dram_tensor("sg_s", [16, 128, 2 * S], BF16, kind=scr_kind).ap()
    hrows_d = nc.dram_tensor("hrows_s", [S, D], F32, kind=scr_kind).ap()
    list_d = nc.dram_tensor("list_s", [NE * CAP + 128, 1], F32, kind=scr_kind).ap()
    yrows_d = nc.dram_tensor("yrows_s", [NE * CAP + 128, D], F32, kind=scr_kind).ap()
    wgu_bf = nc.dram_tensor("wgu_bf", [NE, 4, 128, 8192], BF16, kind="Internal").ap()
    dbg_d = nc.dram_tensor("dbg_s", [128, 16 * 32 + 64 + 64], F32, kind=scr_kind).ap()

    with ExitStack() as st:
        arena = st.enter_context(nc.sbuf_tensor("arena", [128, ARENA], dt.uint8))
        ps = [st.enter_context(nc.psum_tensor("ps%d" % i, [128, 512], F32)) for i in range(8)]
        P = Prog(nc)

        def V(off, shape, d):
            n = 1
            for s_ in shape[1:]:
                n *= s_
            nb = n * _dsize(d)
            assert off % 4 == 0 and off + nb <= ARENA, (off, nb)
            ap = arena[:, off:off + nb].bitcast(d)
            if len(shape) == 3:
                ap = ap.rearrange("p (a b) -> p a b", a=shape[1])
            elif len(shape) == 4:
                ap = ap.rearrange("p (a b c) -> p a b c", a=shape[1], b=shape[2])
            return ap

        class Alloc:
            def __init__(self, base):
                self.off = base

            def __call__(self, shape, d):
                n = 1
                for s_ in shape[1:]:
                    n *= s_
                nb = (n * _dsize(d) + 31) // 32 * 32
                v = V(self.off, shape, d)
                self.off += nb
                return v

        KB = 1024
        CA = Alloc(0)
        ident = CA([128, 128], F32)
        ustr = CA([128, 128], BF16)
        ones_bf = CA([128, 128], BF16)
        eps_t = CA([128, 1], F32)
        ecrow = CA([128, 32], F32)
        tokid = CA([128, 16], F32)
        brB = CA([128, 32], F32)
        logits = CA([128, 16, 32], F32)
        gates4 = CA([128, 64], F32)
        idx_all = CA([128, 64], I32)
        li = CA([128, 96], I32)
        carry = CA([128, 32], F32)
        assert CA.off <= 8 * KB, CA.off
        BASE = 8 * KB

        def bc(ap_row, n):
            return ap_row.broadcast_to([128, n])

        P.dma("sp", lambda e: e.dma_start(out=ident, in_=ident_d), writes=["ident"])
        P.dma("pool", lambda e: e.dma_start(out=ustr, in_=ustr_d), writes=["ustr"])
        P.dve(lambda e: e.memset(ones_bf, 1.0), writes=["ones_bf"])
        P.dve(lambda e: e.memset(eps_t, EPS), writes=["eps_t"])
        P.dma("sp", lambda e: e.dma_start(out=ecrow, in_=bc(ecrow_d, 32)), writes=["ecrow"])
        P.dma("sp", lambda e: e.dma_start(out=tokid, in_=tokid_d), writes=["tokid"])
        P.dma("sp", lambda e: e.dma_start(out=brB, in_=bc(br_d, 32)), writes=["brB"])

        bank_ctr = [0]
        ps_list = [("gu", e_, b_) for e_ in range(NE) for b_ in (1, 3, 5, 7)]
        ps_pos = [0]

        def prestage(n):
            for _ in range(n):
                if ps_pos[0] >= len(ps_list):
                    return
                kind, e_, b_ = ps_list[ps_pos[0]]
                ps_pos[0] += 1
                P.dma("pool", lambda e, e_=e_, b_=b_: e.dma_start(out=wgu_bf[e_, b_ // 2], in_=wgu_d[e_, b_], max_dma_last_dim=8192),
                      writes=["prestage"], semkey=("dma", "prestage"))

        def mm_group(psap, pairs, reads, psname):
            n = len(pairs)
            for i, (l, r) in enumerate(pairs):
                P.pe(lambda e, l=l, r=r, i=i: e.matmul(psap, lhsT=l, rhs=r, start=(i == 0), stop=(i == n - 1)),
                     reads=reads, writes=[psname])

        A = Alloc(BASE)
        xT = A([128, 16, S], BF16)
        ring = [A([128, 8192], BF16) for _ in range(2)]
        PH = A.off
        for kc in range(16):
            P.dma("pool", lambda e, kc=kc: e.dma_start(out=xT[:, kc, :], in_=xT_d[:, kc * S:(kc + 1) * S]),
                  writes=["xT"])

        G = Alloc(PH)
        uT = G([128, 8, S], BF16)
        vg = [G([128, 1024], F32) for _ in range(2)]
        vn = [G([128, 1024], F32) for _ in range(2)]
        vln = [G([128, 1024], BF16) for _ in range(2)]
        lngB = G([128, 1024], F32)
        lnbB = G([128, 1024], F32)
        bsB = G([128, 8, 128], F32)
        wsT = G([128, 8, 128], BF16)
        wsf = G([128, 8, 128], F32)
        trl = G([128, 128], F32)
        stats = [G([128, 4, 6], F32) for _ in range(2)]
        mv = [G([128, 2], F32) for _ in range(2)]
        rstd = [G([128, 1], F32) for _ in range(2)]
        mtmp = [G([128, 4, 128], F32) for _ in range(2)]

        P.dma("sp", lambda e: e.dma_start(out=lngB, in_=bc(slng_d, 1024)), writes=["lngB"])
        P.dma("sp", lambda e: e.dma_start(out=lnbB, in_=bc(slnb_d, 1024)), writes=["lnbB"])
        P.dma("sp", lambda e: e.dma_start(out=bsB.rearrange("p a b -> p (a b)"), in_=bc(sgub_d, 1024)), writes=["bsB"])
        P.dma("sp", lambda e: e.dma_start(out=wsf.rearrange("p a b -> p (a b)"), in_=swt_d), writes=["wsf"])
        P.dma("sp", lambda e: e.dma_start(out=trl, in_=tril_d), writes=["trl"])
        for g in range(8):
            P.dve(lambda e, g=g: e.tensor_tensor(out=wsT[:, g, :], in0=wsf[:, g, :], in1=trl, op=ALU.mult),
                  reads=["wsf", "trl"], writes=["wsT"], nowaw=True)

        def load_ring(slot, src, ncols):
            P.dma("pool", lambda e: e.dma_start(out=ring[slot][:, 0:16 * ncols], in_=src, max_dma_last_dim=8192),
                  writes=["ring%d" % slot])

        def ring3(slot, ncols):
            return ring[slot][:, 0:16 * ncols].rearrange("p (k c) -> p k c", k=16)

        load_ring(0, wz_d[0], 512)
        load_ring(1, wz_d[1], 512)
        for blk in range(2):
            W = ring3(blk, 512)
            for j in range(4):
                c = blk * 4 + j
                for tb in range(4):
                    b = bank_ctr[0] % 8
                    bank_ctr[0] += 1
                    mm_group(ps[b][:, :], [(W[:, kc, j * 128:(j + 1) * 128], xT[:, kc, tb * 512:(tb + 1) * 512]) for kc in range(16)],
                             ["ring%d" % blk, "xT"], "ps%d" % b)
                    P.act(lambda e, b=b, c=c, tb=tb: e.activation(out=uT[:, c, tb * 512:(tb + 1) * 512], in_=ps[b][:, :],
                                                                  func=AF.Gelu_apprx_tanh),
                          reads=["ps%d" % b], writes=["uT"], nowaw=True)
        load_ring(0, wz_d[2], 512)
        load_ring(1, wz_d[3], 512)
        def sgu_a(tt):
            p2 = tt % 2
            for vb in range(2):
                b = (tt % 2) * 2 + vb
                W = ring3(vb, 512)
                mm_group(ps[b][:, :], [(xT[:, kc, tt * 128:(tt + 1) * 128], W[:, kc, :]) for kc in range(16)],
                         ["ring%d" % vb, "xT"], "ps%d" % b)
                P.act(lambda e, b=b, vb=vb, p2=p2: e.activation(out=vg[p2][:, vb * 512:(vb + 1) * 512], in_=ps[b][:, :],
                                                                func=AF.Gelu_apprx_tanh),
                      reads=["ps%d" % b], writes=["vg%d" % p2], nowaw=True)
            for vb in range(2):
                P.dve(lambda e, vb=vb, p2=p2: e.bn_stats(out=stats[p2][:, vb, :], in_=vg[p2][:, vb * 512:(vb + 1) * 512]),
                      reads=["vg%d" % p2], writes=["stats%d" % p2], nowaw=True)
            P.dve(lambda e, p2=p2: e.bn_aggr(out=mv[p2], in_=stats[p2][:, 0:2, :]), reads=["stats%d" % p2], writes=["mv%d" % p2])
            P.act(lambda e, p2=p2: e.activation(out=rstd[p2], in_=mv[p2][:, 1:2], func=AF.Sqrt, bias=eps_t[:, 0:1], scale=1.0),
                  reads=["mv%d" % p2, "eps_t"], writes=["rstd%d" % p2])
            P.dve(lambda e, p2=p2: e.reciprocal(out=rstd[p2], in_=rstd[p2]), reads=["rstd%d" % p2], writes=["rstd%d" % p2])
            P.dve(lambda e, p2=p2: e.tensor_scalar(out=vn[p2], in0=vg[p2], scalar1=mv[p2][:, 0:1], scalar2=rstd[p2][:, 0:1],
                                                   op0=ALU.subtract, op1=ALU.mult),
                  reads=["vg%d" % p2, "mv%d" % p2, "rstd%d" % p2], writes=["vn%d" % p2])
            P.dve(lambda e, p2=p2: e.tensor_tensor(out=vn[p2], in0=vn[p2], in1=lngB, op=ALU.mult),
                  reads=["vn%d" % p2, "lngB"], writes=["vn%d" % p2])
            P.dve(lambda e, p2=p2: e.tensor_tensor(out=vln[p2], in0=vn[p2], in1=lnbB, op=ALU.add),
                  reads=["vn%d" % p2, "lnbB"], writes=["vln%d" % p2])

        def sgu_b(tt):
            p2 = tt % 2
            for half in range(2):
                b = 4 + (tt % 2) * 2 + half
                for gg in range(4):
                    g = half * 4 + gg
                    P.pe(lambda e, b=b, g=g, gg=gg, p2=p2: e.matmul(ps[b][:, gg * 128:(gg + 1) * 128], lhsT=vln[p2][:, g * 128:(g + 1) * 128],
                                                                    rhs=wsT[:, g, :], start=True, stop=True),
                         reads=["vln%d" % p2, "wsT"], writes=["ps%d" % b], nowaw=True)
                P.dve(lambda e, b=b, half=half, p2=p2: e.tensor_tensor(out=mtmp[p2], in0=ps[b][:, :].rearrange("p (a b) -> p a b", a=4),
                                                                       in1=bsB[:, half * 4:(half + 1) * 4, :], op=ALU.add),
                      reads=["ps%d" % b, "bsB"], writes=["mtmp%d" % p2])
                P.dve(lambda e, half=half, p2=p2, tt=tt: e.tensor_tensor(out=uT[:, half * 4:(half + 1) * 4, tt * 128:(tt + 1) * 128], in0=mtmp[p2],
                                                                        in1=uT[:, half * 4:(half + 1) * 4, tt * 128:(tt + 1) * 128], op=ALU.mult),
                      reads=["mtmp%d" % p2, "uT"], writes=["uT"], nowaw=True)
        for tt in range(16):
            sgu_a(tt)
            if tt >= 1:
                sgu_b(tt - 1)
        sgu_b(15)
        P.dma("sp", lambda e: e.dma_start(out=ysgu_d, in_=uT.rearrange("p a b -> p (a b)")), reads=["uT"], writes=["ysgu_d"])
        P.barrier()

        T = Alloc(PH + 32 * KB)
        qT = [T([128, S], BF16) for _ in range(2)]
        kT = [T([128, S], BF16) for _ in range(2)]
        vS = [T([128, 16, 128], BF16) for _ in range(2)]
        bmt = [T([128, 256], F32) for _ in range(2)]
        sbm = [T([128, 256], F32) for _ in range(3)]
        pT = [T([128, 256], BF16) for _ in range(3)]
        accN = T([128, S], F32)
        accD = T([128, S], F32)
        yout = [T([128, S], BF16) for _ in range(2)]
        sgt = [T([128, 512], BF16) for _ in range(4)]
        assert T.off <= ARENA, T.off

        units = [(h, g) for h in range(NH) for g in range(NG)]
        rs = [0]
        load_ring(0, wa_d[0], 384)
        sbi = [0]
        for ui, (h, g) in enumerate(units):
            r = DIL[g]
            L = S // r
            nb = L // 128
            slot = ui % 2
            par = ui % 2
            if ui + 1 < len(units):
                load_ring((ui + 1) % 2, wa_d[ui + 1], 384)
            P.dma("sp", lambda e, ui=ui, par=par: e.dma_start(out=bmt[par], in_=bm_d[ui]), writes=["bmt%d" % par])
            prestage(3)
            W = ring3(slot, 384)
            q3 = qT[par].rearrange("p (c m) -> p c m", c=r)
            k3 = kT[par].rearrange("p (c m) -> p c m", c=r)
            for which, dst3 in ((0, q3), (1, k3)):
                for tb in range(4):
                    b = tb % 2
                    mm_group(ps[b][:, :], [(W[:, kc, which * 128:(which + 1) * 128], xT[:, kc, tb * 512:(tb + 1) * 512]) for kc in range(16)],
                             ["ring%d" % slot, "xT"], "ps%d" % b)
                    mw = 512 // r
                    dname = ("qT%d" if which == 0 else "kT%d") % par
                    P.act(lambda e, b=b, dst3=dst3, tb=tb, mw=mw, r=r: e.copy(out=dst3[:, :, tb * mw:(tb + 1) * mw],
                                                                             in_=ps[b][:, :].rearrange("p (m c) -> p c m", c=r)),
                          reads=["ps%d" % b], writes=[dname], nowaw=True)
            for tq in range(4):
                b = tq % 2
                for t4 in range(4):
                    ti = tq * 4 + t4
                    c, n = ti // nb, ti % nb
                    t0 = c + r * 128 * n
                    for kc in range(16):
                        lhs = xT[:, kc, t0:t0 + r * 127 + 1:r]
                        P.pe(lambda e, b=b, t4=t4, lhs=lhs, kc=kc, W=W: e.matmul(ps[b][:, t4 * 128:(t4 + 1) * 128], lhsT=lhs, rhs=W[:, kc, 256:384],
                                                                               start=(kc == 0), stop=(kc == 15)),
                             reads=["ring%d" % slot, "xT"], writes=["ps%d" % b], nowaw=True)
                P.dve(lambda e, b=b, tq=tq, par=par: e.tensor_copy(out=vS[par][:, tq * 4:(tq + 1) * 4, :],
                                                                   in_=ps[b][:, :].rearrange("p (a b) -> p a b", a=4)),
                      reads=["ps%d" % b], writes=["vS%d" % par], nowaw=True)
            ob_started = set()
            ob_writes = {}

            def flush_bank(ob, g=g, r=r, L=L):
                nbk = 4 + ob % 2
                dbk = 6 + ob % 2
                if L >= 512:
                    c0 = (512 * ob) // L
                    m0 = (512 * ob) % L
                    vN = accN.rearrange("p (m c) -> p c m", c=r)[:, c0, m0:m0 + 512]
                    vD = accD.rearrange("p (m c) -> p c m", c=r)[:, c0, m0:m0 + 512]
                    pN = ps[nbk][:, :]
                    pD = ps[dbk][:, :]
                else:
                    nres = 512 // L
                    c0 = ob * nres
                    vN = accN.rearrange("p (m c) -> p c m", c=r)[:, c0:c0 + nres, :]
                    vD = accD.rearrange("p (m c) -> p c m", c=r)[:, c0:c0 + nres, :]
                    pN = ps[nbk][:, :].rearrange("p (c m) -> p c m", c=nres)
                    pD = ps[dbk][:, :].rearrange("p (c m) -> p c m", c=nres)
                if g == 0:
                    P.dve(lambda e: e.tensor_copy(out=vN, in_=pN), reads=["ps%d" % nbk], writes=["accN"], nowaw=True)
                    P.dve(lambda e: e.tensor_copy(out=vD, in_=pD), reads=["ps%d" % dbk], writes=["accD"], nowaw=True)
                else:
                    P.dve(lambda e: e.tensor_tensor(out=vN, in0=pN, in1=vN, op=ALU.add), reads=["ps%d" % nbk, "accN"], writes=["accN"], nowaw=True)
                    P.dve(lambda e: e.tensor_tensor(out=vD, in0=pD, in1=vD, op=ALU.add), reads=["ps%d" % dbk, "accD"], writes=["accD"], nowaw=True)

            steps = [(c, kt, 256 if kt < nb - 1 else 128) for c in range(r) for kt in range(nb)]
            sb0 = sbi[0]
            sbi[0] += len(steps)
            cur_ob_box = [0]

            def emit_S(i, steps=steps, sb0=sb0, k3=k3, q3=q3, par=par):
                c, kt, ncols = steps[i]
                sb_ = 2 + (sb0 + i) % 2
                si = (sb0 + i) % 3
                P.pe(lambda e: e.matmul(ps[sb_][:, 0:ncols], lhsT=k3[:, c, kt * 128:(kt + 1) * 128],
                                        rhs=q3[:, c, kt * 128:kt * 128 + ncols], start=True, stop=True),
                     reads=["kT%d" % par, "qT%d" % par], writes=["ps%d" % sb_])
                P.dve(lambda e: e.scalar_tensor_tensor(out=sbm[si][:, 0:ncols], in0=ps[sb_][:, 0:ncols], scalar=SCALE,
                                                       in1=bmt[par][:, 0:ncols], op0=ALU.mult, op1=ALU.add),
                      reads=["ps%d" % sb_, "bmt%d" % par], writes=["sbm%d" % si])
                P.act(lambda e: e.activation(out=pT[si][:, 0:ncols], in_=sbm[si][:, 0:ncols], func=AF.Exp),
                      reads=["sbm%d" % si], writes=["pT%d" % si])

            def emit_PV(i, steps=steps, sb0=sb0, par=par, L=L, nb=nb):
                c, kt, ncols = steps[i]
                si = (sb0 + i) % 3
                base = c * L + kt * 128
                if ncols == 256 and (base % 512) == 384:
                    pieces = [(base, 0, 128), (base + 128, 128, 128)]
                else:
                    pieces = [(base, 0, ncols)]
                for (lb, poff, pn) in pieces:
                    ob = lb // 512
                    if ob != cur_ob_box[0]:
                        flush_bank(cur_ob_box[0])
                        cur_ob_box[0] = ob
                    col = lb % 512
                    first = ob not in ob_started
                    ob_started.add(ob)
                    nbk = 4 + ob % 2
                    dbk = 6 + ob % 2
                    ti = c * nb + kt
                    P.pe(lambda e, nbk=nbk, col=col, pn=pn, ti=ti, poff=poff, first=first: e.matmul(
                        ps[nbk][:, col:col + pn], lhsT=vS[par][:, ti, :], rhs=pT[si][:, poff:poff + pn], start=first, stop=True),
                        reads=["vS%d" % par, "pT%d" % si], writes=["ps%d" % nbk], nowaw=True)
                    P.pe(lambda e, dbk=dbk, col=col, pn=pn, poff=poff, first=first: e.matmul(
                        ps[dbk][:, col:col + pn], lhsT=ones_bf, rhs=pT[si][:, poff:poff + pn], start=first, stop=True),
                        reads=["ones_bf", "pT%d" % si], writes=["ps%d" % dbk], nowaw=True)

            for i in range(len(steps)):
                emit_S(i)
                if i >= 1:
                    emit_PV(i - 1)
            emit_PV(len(steps) - 1)
            cur_ob = cur_ob_box[0]
            flush_bank(cur_ob)
            if g == NG - 1:
                yp = h % 2
                P.dve(lambda e: e.reciprocal(out=accD, in_=accD), reads=["accD"], writes=["accD"])
                P.dve(lambda e, yp=yp: e.tensor_tensor(out=yout[yp], in0=accN, in1=accD, op=ALU.mult),
                      reads=["accN", "accD"], writes=["yout%d" % yp])
                P.dma("sp", lambda e, yp=yp, h=h: e.dma_start(out=yattn_d[h], in_=yout[yp]), reads=["yout%d" % yp], writes=["yattn_d"], semkey=("st", "yout%d" % yp))

        gslot = len(units) % 2
        load_ring(gslot, wg_d[0], 512)
        sgi = 0
        for blk in range(8):
            slot = (gslot + blk) % 2
            if blk + 1 < 8:
                load_ring((slot + 1) % 2, wg_d[blk + 1], 512)
            prestage(2)
            W = ring3(slot, 512)
            for j in range(4):
                ci = blk * 4 + j
                n_, dc = ci // 16, ci % 16
                for tb in range(4):
                    b = bank_ctr[0] % 8
                    bank_ctr[0] += 1
                    mm_group(ps[b][:, :], [(W[:, kc, j * 128:(j + 1) * 128], xT[:, kc, tb * 512:(tb + 1) * 512]) for kc in range(16)],
                             ["ring%d" % slot, "xT"], "ps%d" % b)
                    s4 = sgi % 4
                    sgi += 1
                    P.act(lambda e, b=b, s4=s4: e.activation(out=sgt[s4], in_=ps[b][:, :], func=AF.Sigmoid),
                          reads=["ps%d" % b], writes=["sgt%d" % s4])
                    P.dma("sp", lambda e, s4=s4, dc=dc, n_=n_, tb=tb: e.dma_start(
                        out=sg_d[dc][:, n_ * S + tb * 512: n_ * S + (tb + 1) * 512], in_=sgt[s4]),
                        reads=["sgt%d" % s4], writes=["sg_d"], semkey=("st", "sgt%d" % s4))

        P.barrier()
        B = Alloc(BASE)
        gT = B([128, 16, S], BF16)
        yat = B([128, 8, S], BF16)
        ysg = B([128, 8, S], BF16)
        wbr = [B([128, 2, 8, 128], BF16) for _ in range(2)]
        sgl = [B([128, 2, S], BF16) for _ in range(2)]
        tg = [B([128, 512], F32) for _ in range(4)]
        PB2 = B.off
        P.dma("sp", lambda e: e.dma_start(out=yat, in_=yattn_d.rearrange("h p t -> p h t")), reads=["yattn_d"], writes=["yat"])
        P.dma("sp", lambda e: e.dma_start(out=ysg.rearrange("p a b -> p (a b)"), in_=ysgu_d), reads=["ysgu_d"], writes=["ysg"])

        def load_b1(dc):
            s2 = dc % 2
            P.dma("pool", lambda e: e.dma_start(out=wbr[s2].rearrange("p a b c -> p (a b c)"), in_=wbr_d[dc]), writes=["wbr%d" % s2])
            P.dma("sp", lambda e: e.dma_start(out=sgl[s2].rearrange("p a b -> p (a b)"), in_=sg_d[dc]), reads=["sg_d"], writes=["sgl%d" % s2])

        load_b1(0)
        tgi = 0
        for dc in range(16):
            s2 = dc % 2
            if dc + 1 < 16:
                load_b1(dc + 1)
            prestage(1)
            for tb in range(4):
                b0 = (bank_ctr[0] % 4) * 2
                bank_ctr[0] += 1
                b1 = b0 + 1
                mm_group(ps[b0][:, :], [(wbr[s2][:, 0, kc, :], yat[:, kc, tb * 512:(tb + 1) * 512]) for kc in range(8)],
                         ["wbr%d" % s2, "yat"], "ps%d" % b0)
                mm_group(ps[b1][:, :], [(wbr[s2][:, 1, kc, :], ysg[:, kc, tb * 512:(tb + 1) * 512]) for kc in range(8)],
                         ["wbr%d" % s2, "ysg"], "ps%d" % b1)
                ta, tb_ = tgi % 4, (tgi + 1) % 4
                tgi += 2
                P.dve(lambda e, b0=b0, ta=ta, s2=s2, tb=tb: e.tensor_tensor(out=tg[ta], in0=ps[b0][:, :], in1=sgl[s2][:, 0, tb * 512:(tb + 1) * 512], op=ALU.mult),
                      reads=["ps%d" % b0, "sgl%d" % s2], writes=["tg%d" % ta])
                P.dve(lambda e, b1=b1, tb_=tb_, s2=s2, tb=tb: e.tensor_tensor(out=tg[tb_], in0=ps[b1][:, :], in1=sgl[s2][:, 1, tb * 512:(tb + 1) * 512], op=ALU.mult),
                      reads=["ps%d" % b1, "sgl%d" % s2], writes=["tg%d" % tb_])
                P.dve(lambda e, ta=ta, tb_=tb_, dc=dc, tb=tb: e.tensor_tensor(out=gT[:, dc, tb * 512:(tb + 1) * 512], in0=tg[ta], in1=tg[tb_], op=ALU.add),
                      reads=["tg%d" % ta, "tg%d" % tb_], writes=["gT"], nowaw=True)

        P.barrier()
        C2 = Alloc(BASE + 64 * KB)
        wout = C2([128, 16, D], BF16)
        xt = [C2([128, D], F32) for _ in range(2)]
        pre = [C2([128, D], F32) for _ in range(2)]
        g1B = C2([128, D], F32)
        b1B = C2([128, D], F32)
        hTt = [C2([128, 16, 128], F32) for _ in range(2)]
        wr = C2([128, 16, 32], F32)
        st1 = [C2([128, 4, 6], F32) for _ in range(2)]
        mv1 = [C2([128, 2], F32) for _ in range(2)]
        rs1 = [C2([128, 1], F32) for _ in range(2)]
        top8 = [C2([128, 8], F32) for _ in range(2)]
        maskb = [C2([128, 32], BF16) for _ in range(2)]
        negm = [C2([128, 1], F32) for _ in range(2)]
        e4 = [C2([128, 4], F32) for _ in range(2)]
        esum = [C2([128, 1], F32) for _ in range(2)]
        posg = [C2([128, 32], F32) for _ in range(2)]
        junk = [C2([128, 32], F32) for _ in range(2)]
        idxf = [C2([128, 4], F32) for _ in range(2)]
        zl = C2([128, 97], F32)
        assert C2.off <= ARENA, C2.off

        for dc in range(16):
            P.dma("pool", lambda e, dc=dc: e.dma_start(out=wout[:, dc, :], in_=wout_d[:, dc * D:(dc + 1) * D]), writes=["wout"])
        P.dma("sp", lambda e: e.dma_start(out=g1B, in_=bc(ln1g_d, D)), writes=["g1B"])
        P.dma("sp", lambda e: e.dma_start(out=b1B, in_=bc(ln1b_d, D)), writes=["b1B"])
        P.dma("sp", lambda e: e.dma_start(out=wr.rearrange("p a b -> p (a b)"), in_=wr_d), writes=["wr"])
        P.dve(lambda e: e.memset(zl, 0.0), writes=["zl"])
        P.dma("sp", lambda e: e.dma_start(out=list_d.rearrange("(p r) o -> p (r o)", p=128), in_=zl), reads=["zl"], writes=["list_d"])
        P.dve(lambda e: e.tensor_copy(out=carry, in_=ecrow), reads=["ecrow"], writes=["carry"])

        def ln_rows(src, p2, stt, mvv, rss, tagp):
            for cb in range(4):
                P.dve(lambda e, cb=cb: e.bn_stats(out=stt[:, cb, :], in_=src[:, cb * 512:(cb + 1) * 512]),
                      reads=[tagp + "src%d" % p2], writes=[tagp + "st%d" % p2], nowaw=True)
            P.dve(lambda e: e.bn_aggr(out=mvv, in_=stt), reads=[tagp + "st%d" % p2], writes=[tagp + "mv%d" % p2])
            P.act(lambda e: e.activation(out=rss, in_=mvv[:, 1:2], func=AF.Sqrt, bias=eps_t[:, 0:1], scale=1.0),
                  reads=[tagp + "mv%d" % p2, "eps_t"], writes=[tagp + "rs%d" % p2])
            P.dve(lambda e: e.reciprocal(out=rss, in_=rss), reads=[tagp + "rs%d" % p2], writes=[tagp + "rs%d" % p2])

        P.dma("sp", lambda e: e.dma_start(out=xt[0], in_=x_d[0:128, :]), writes=["xt0"])
        def b2_part1(tt):
            prestage(2)
            p2 = tt % 2
            if tt + 1 < 16:
                P.dma("sp", lambda e, tt=tt: e.dma_start(out=xt[(tt + 1) % 2], in_=x_d[(tt + 1) * 128:(tt + 2) * 128, :]),
                      writes=["xt%d" % ((tt + 1) % 2)])
            for cb in range(4):
                b = cb
                mm_group(ps[b][:, :], [(gT[:, dc, tt * 128:(tt + 1) * 128], wout[:, dc, cb * 512:(cb + 1) * 512]) for dc in range(16)],
                         ["gT", "wout"], "ps%d" % b)
                P.dve(lambda e, b=b, cb=cb, p2=p2: e.scalar_tensor_tensor(out=pre[p2][:, cb * 512:(cb + 1) * 512], in0=xt[p2][:, cb * 512:(cb + 1) * 512],
                                                                          scalar=ALPHA, in1=ps[b][:, :], op0=ALU.mult, op1=ALU.add),
                      reads=["ps%d" % b, "xt%d" % p2], writes=["L1src%d" % p2], nowaw=True)
            ln_rows(pre[p2], p2, st1[p2], mv1[p2], rs1[p2], "L1")
            P.dve(lambda e, p2=p2: e.scalar_tensor_tensor(out=pre[p2], in0=pre[p2], scalar=mv1[p2][:, 0:1], in1=g1B, op0=ALU.subtract, op1=ALU.mult),
                  reads=["L1src%d" % p2, "L1mv%d" % p2, "g1B"], writes=["L1src%d" % p2])
            P.dve(lambda e, p2=p2: e.scalar_tensor_tensor(out=pre[p2], in0=pre[p2], scalar=rs1[p2][:, 0:1], in1=b1B, op0=ALU.mult, op1=ALU.add),
                  reads=["L1src%d" % p2, "L1rs%d" % p2, "b1B"], writes=["L1src%d" % p2])
            P.dma("sp", lambda e, p2=p2, tt=tt: e.dma_start(out=hrows_d[tt * 128:(tt + 1) * 128, :], in_=pre[p2]),
                  reads=["L1src%d" % p2], writes=["hrows_d"], semkey=("st", "pre%d" % p2))

        def b2_part2(tt):
            p2 = tt % 2
            for q4 in range(4):
                b = 4 + q4 % 2
                for j in range(4):
                    kc = q4 * 4 + j
                    P.pe(lambda e, b=b, j=j, kc=kc, p2=p2: e.transpose(out=ps[b][:, j * 128:(j + 1) * 128], in_=pre[p2][:, kc * 128:(kc + 1) * 128], identity=ident),
                         reads=["L1src%d" % p2, "ident"], writes=["ps%d" % b], nowaw=True)
                P.act(lambda e, b=b, q4=q4, p2=p2: e.copy(out=hTt[p2][:, q4 * 4:(q4 + 1) * 4, :], in_=ps[b][:, :].rearrange("p (a b) -> p a b", a=4)),
                      reads=["ps%d" % b], writes=["hT%d" % p2], nowaw=True)
            mm_group(ps[6][:, 0:32], [(hTt[p2][:, kc, :], wr[:, kc, :]) for kc in range(16)], ["hT%d" % p2, "wr"], "ps6")
            P.dve(lambda e, tt=tt: e.tensor_tensor(out=logits[:, tt, :], in0=ps[6][:, 0:32], in1=brB, op=ALU.add),
                  reads=["ps6", "brB"], writes=["lg%d" % p2])
            lg = logits[:, tt, :]
            P.dve(lambda e, p2=p2, lg=lg: e.max(out=top8[p2], in_=lg), reads=["lg%d" % p2], writes=["top8%d" % p2])
            P.dve(lambda e, p2=p2, lg=lg: e.tensor_scalar(out=maskb[p2], in0=lg, scalar1=top8[p2][:, 3:4], scalar2=None, op0=ALU.is_ge),
                  reads=["lg%d" % p2, "top8%d" % p2], writes=["maskb%d" % p2])
            P.dve(lambda e, p2=p2: e.tensor_scalar(out=negm[p2], in0=top8[p2][:, 0:1], scalar1=-1.0, scalar2=None, op0=ALU.mult),
                  reads=["top8%d" % p2], writes=["negm%d" % p2])
            P.act(lambda e, p2=p2: e.activation(out=e4[p2], in_=top8[p2][:, 0:4], func=AF.Exp, bias=negm[p2][:, 0:1], scale=1.0),
                  reads=["top8%d" % p2, "negm%d" % p2], writes=["e4%d" % p2])
            P.dve(lambda e, p2=p2: e.reduce_sum(out=esum[p2], in_=e4[p2], axis=mybir.AxisListType.X),
                  reads=["e4%d" % p2], writes=["esum%d" % p2])
            P.dve(lambda e, p2=p2: e.reciprocal(out=esum[p2], in_=esum[p2]), reads=["esum%d" % p2], writes=["esum%d" % p2])
            P.dve(lambda e, p2=p2, tt=tt: e.tensor_scalar(out=gates4[:, tt * 4:(tt + 1) * 4], in0=e4[p2], scalar1=esum[p2][:, 0:1], scalar2=None, op0=ALU.mult),
                  reads=["e4%d" % p2, "esum%d" % p2], writes=["gates4"], nowaw=True)
            P.pe(lambda e, p2=p2: e.matmul(ps[7][:, 0:32], lhsT=ustr, rhs=maskb[p2], start=True, stop=True),
                 reads=["ustr", "maskb%d" % p2], writes=["ps7a"])
            P.pe(lambda e, p2=p2: e.matmul(ps[7][:, 32:64], lhsT=ones_bf, rhs=maskb[p2], start=True, stop=True),
                 reads=["ones_bf", "maskb%d" % p2], writes=["ps7b"])
            P.dve(lambda e, p2=p2: e.tensor_tensor(out=posg[p2], in0=ps[7][:, 0:32], in1=carry, op=ALU.add),
                  reads=["ps7a", "carry"], writes=["posg%d" % p2])
            P.dve(lambda e: e.tensor_tensor(out=carry, in0=ps[7][:, 32:64], in1=carry, op=ALU.add),
                  reads=["ps7b", "carry"], writes=["carry"])
            for k in range(4):
                P.dve(lambda e, p2=p2, k=k, lg=lg: e.scalar_tensor_tensor(out=junk[p2], in0=lg, scalar=top8[p2][:, k:k + 1], in1=posg[p2],
                                                                          op0=ALU.is_equal, op1=ALU.mult),
                      reads=["lg%d" % p2, "top8%d" % p2, "posg%d" % p2], writes=["junk%d" % p2])
                P.dve(lambda e, p2=p2, k=k: e.reduce_sum(out=idxf[p2][:, k:k + 1], in_=junk[p2], axis=mybir.AxisListType.X),
                      reads=["junk%d" % p2], writes=["idxf%d" % p2], nowaw=True)
            P.dve(lambda e, p2=p2, tt=tt: e.tensor_scalar(out=idx_all[:, tt * 4:(tt + 1) * 4], in0=idxf[p2], scalar1=0.0, scalar2=float(NE * CAP + 127),
                                                          op0=ALU.max, op1=ALU.min),
                  reads=["idxf%d" % p2], writes=["idx_all"], nowaw=True)
            for k in range(4):
                P.dma("pool", lambda e, tt=tt, k=k: e.indirect_dma_start(
                    out=list_d, out_offset=bass.IndirectOffsetOnAxis(ap=idx_all[:, tt * 4 + k:tt * 4 + k + 1], axis=0),
                    in_=tokid[:, tt:tt + 1], in_offset=None),
                    reads=["idx_all", "tokid", "list_d"], writes=["list_d2"])
        for tt in range(16):
            b2_part1(tt)
            if tt >= 1:
                b2_part2(tt - 1)
        b2_part2(15)
        if DEBUG:
            P.dma("sp", lambda e: e.dma_start(out=dbg_d[:, 0:512], in_=logits.rearrange("p a b -> p (a b)")), reads=["lg0", "lg1"], writes=["dbg"])
            P.dma("sp", lambda e: e.dma_start(out=dbg_d[:, 512:576], in_=gates4), reads=["gates4"], writes=["dbg"])
            P.dma("sp", lambda e: e.dma_start(out=dbg_d[:, 576:640], in_=idx_all.bitcast(F32)), reads=["idx_all"], writes=["dbg"])

        P.barrier()
        M = Alloc(BASE)
        lf = M([128, 128], F32)
        bgu = M([128, NE, 32], F32)
        Xe = [M([128, D], F32) for _ in range(3)]
        XeT = [M([128, 16, CAP], BF16) for _ in range(2)]
        wgu = [M([128, 8192], BF16) for _ in range(3)]
        wdn = [M([128, 8192], BF16) for _ in range(2)]
        actT = [M([128, 16, CAP], BF16) for _ in range(2)]
        gl = [M([128, CAP], F32) for _ in range(2)]
        sgm = [M([128, CAP], F32) for _ in range(2)]
        ln_ = [M([128, CAP], F32) for _ in range(2)]
        ypc = [M([128, 512], F32) for _ in range(4)]
        bdB = [M([128, D], F32) for _ in range(2)]
        assert M.off <= ARENA, M.off

        P.dma("sp", lambda e: e.dma_start(out=lf[0:96, :], in_=list_d[0:NE * CAP, :].rearrange("(n p) o -> n (p o)", p=128)),
              reads=["list_d", "list_d2"], writes=["lf"])
        P.dma("sp", lambda e: e.dma_start(out=bgu.rearrange("p a b -> p (a b)"), in_=bgu_d), writes=["bgu"])
        P.pe(lambda e: e.transpose(out=ps[0][:, 0:96], in_=lf[0:96, :], identity=ident[0:96, 0:96]), reads=["lf", "ident"], writes=["ps0"])
        P.dve(lambda e: e.tensor_scalar(out=li, in0=ps[0][:, 0:96], scalar1=0.0, scalar2=float(S - 1), op0=ALU.max, op1=ALU.min),
              reads=["ps0"], writes=["li"])

        wstream = []
        for e_ in range(NE):
            for blk in range(8):
                wstream.append(("gu", e_, blk))
            for db in range(4):
                wstream.append(("dn", e_, db))
        cnt = {"gu": 0, "dn": 0}
        slot_of = {}
        issued = [0]

        consumed = {"gu": 0, "dn": 0}
        capk = {"gu": 3, "dn": 2}

        def pump():
            while issued[0] < len(wstream):
                kind, e_, bi = wstream[issued[0]]
                if cnt[kind] - consumed[kind] >= capk[kind]:
                    break
                if kind == "gu":
                    s_ = cnt["gu"] % 3
                    if bi % 2 == 1:
                        P.dma("sp", lambda e, s_=s_, e_=e_, bi=bi: e.dma_start(out=wgu[s_], in_=wgu_bf[e_, bi // 2]), writes=["wgu%d" % s_])
                    else:
                        P.dma("pool", lambda e, s_=s_, e_=e_, bi=bi: e.dma_start(out=wgu[s_], in_=wgu_d[e_, bi], max_dma_last_dim=8192), writes=["wgu%d" % s_])
                else:
                    s_ = cnt["dn"] % 2
                    P.dma("pool", lambda e, s_=s_, e_=e_, bi=bi: e.dma_start(out=wdn[s_], in_=wd_d[e_, bi], max_dma_last_dim=8192), writes=["wdn%d" % s_])
                cnt[kind] += 1
                slot_of[issued[0]] = s_
                issued[0] += 1

        def gather_x(e_):
            for s3 in range(NST):
                P.dma("pool", lambda e, e_=e_, s3=s3: e.indirect_dma_start(
                    out=Xe[s3], out_offset=None, in_=hrows_d,
                    in_offset=bass.IndirectOffsetOnAxis(ap=li[:, e_ * NST + s3:e_ * NST + s3 + 1], axis=0)),
                    reads=["li", "hrows_d"], writes=["Xe%d" % s3])

        gather_x(0)
        wi = 0
        tci = 0
        yi = 0
        for e_ in range(NE):
            xp = e_ % 2
            P.dma("sp", lambda e, e_=e_, xp=xp: e.dma_start(out=bdB[xp], in_=bc(bd_d[e_:e_ + 1, :], D)), writes=["bdB%d" % xp])
            for s3 in range(NST):
                for q4 in range(4):
                    b = tci % 2
                    tci += 1
                    for j in range(4):
                        kc = q4 * 4 + j
                        P.pe(lambda e, b=b, j=j, kc=kc, s3=s3: e.transpose(out=ps[b][:, j * 128:(j + 1) * 128], in_=Xe[s3][:, kc * 128:(kc + 1) * 128], identity=ident),
                             reads=["Xe%d" % s3, "ident"], writes=["ps%d" % b], nowaw=True)
                    P.act(lambda e, b=b, q4=q4, s3=s3, xp=xp: e.copy(out=XeT[xp][:, q4 * 4:(q4 + 1) * 4, s3 * 128:(s3 + 1) * 128],
                                                                     in_=ps[b][:, :].rearrange("p (a b) -> p a b", a=4)),
                          reads=["ps%d" % b], writes=["XeT%d" % xp], nowaw=True)
            if e_ + 1 < NE:
                gather_x(e_ + 1)
            for blk in range(8):
                pump()
                s_ = slot_of[wi]
                wi += 1
                W = wgu[s_].rearrange("p (k c) -> p k c", k=16)
                for mm_ in range(2):
                    m = blk * 2 + mm_
                    pg = 2 + (m % 2) * 2
                    pl = pg + 1
                    a2 = m % 2
                    mm_group(ps[pg][:, 0:CAP], [(W[:, kc, mm_ * 256:mm_ * 256 + 128], XeT[xp][:, kc, :]) for kc in range(16)],
                             ["wgu%d" % s_, "XeT%d" % xp], "ps%d" % pg)
                    mm_group(ps[pl][:, 0:CAP], [(W[:, kc, mm_ * 256 + 128:mm_ * 256 + 256], XeT[xp][:, kc, :]) for kc in range(16)],
                             ["wgu%d" % s_, "XeT%d" % xp], "ps%d" % pl)
                    P.dve(lambda e, pg=pg, a2=a2, e_=e_, m=m: e.tensor_scalar(out=gl[a2], in0=ps[pg][:, 0:CAP], scalar1=bgu[:, e_, m:m + 1], scalar2=7.0,
                                                                              op0=ALU.add, op1=ALU.min),
                          reads=["ps%d" % pg, "bgu"], writes=["gl%d" % a2])
                    P.act(lambda e, a2=a2: e.activation(out=sgm[a2], in_=gl[a2], func=AF.Sigmoid, scale=1.702),
                          reads=["gl%d" % a2], writes=["sgm%d" % a2])
                    P.dve(lambda e, pl=pl, a2=a2, e_=e_, m=m: e.tensor_scalar(out=ln_[a2], in0=ps[pl][:, 0:CAP], scalar1=bgu[:, e_, 16 + m:17 + m], scalar2=7.0,
                                                                              op0=ALU.add, op1=ALU.min),
                          reads=["ps%d" % pl, "bgu"], writes=["ln%d" % a2])
                    P.dve(lambda e, a2=a2: e.tensor_scalar(out=ln_[a2], in0=ln_[a2], scalar1=-7.0, scalar2=1.0, op0=ALU.max, op1=ALU.add),
                          reads=["ln%d" % a2], writes=["ln%d" % a2])
                    P.dve(lambda e, a2=a2: e.tensor_tensor(out=gl[a2], in0=gl[a2], in1=sgm[a2], op=ALU.mult),
                          reads=["gl%d" % a2, "sgm%d" % a2], writes=["gl%d" % a2])
                    P.dve(lambda e, a2=a2, xp=xp, m=m: e.tensor_tensor(out=actT[xp][:, m, :], in0=gl[a2], in1=ln_[a2], op=ALU.mult),
                          reads=["gl%d" % a2, "ln%d" % a2], writes=["actT%d" % xp], nowaw=True)
                consumed["gu"] += 1
                pump()
            for db in range(4):
                pump()
                s_ = slot_of[wi]
                wi += 1
                Wd = wdn[s_].rearrange("p (k c) -> p k c", k=16)
                for s3 in range(NST):
                    b = 6 + yi % 2
                    y4 = yi % 4
                    yi += 1
                    mm_group(ps[b][:, :], [(actT[xp][:, hc, s3 * 128:(s3 + 1) * 128], Wd[:, hc, :]) for hc in range(16)],
                             ["wdn%d" % s_, "actT%d" % xp], "ps%d" % b)
                    P.dve(lambda e, b=b, y4=y4, xp=xp, db=db: e.tensor_tensor(out=ypc[y4], in0=ps[b][:, :], in1=bdB[xp][:, db * 512:(db + 1) * 512], op=ALU.add),
                          reads=["ps%d" % b, "bdB%d" % xp], writes=["ypc%d" % y4])
                    r0 = e_ * CAP + s3 * 128
                    P.dma("sp", lambda e, y4=y4, r0=r0, db=db: e.dma_start(out=yrows_d[r0:r0 + 128, db * 512:(db + 1) * 512], in_=ypc[y4]),
                          reads=["ypc%d" % y4], writes=["yrows_d"], semkey=("st", "ypc%d" % y4))
                consumed["dn"] += 1
                pump()

        P.barrier()
        F = Alloc(BASE)
        ht = [F([128, D], F32) for _ in range(2)]
        yk = [F([128, D], F32) for _ in range(4)]
        acc = [F([128, D], F32) for _ in range(2)]
        g2B = F([128, D], F32)
        b2B = F([128, D], F32)
        st2 = [F([128, 4, 6], F32) for _ in range(2)]
        mv2 = [F([128, 2], F32) for _ in range(2)]
        rs2 = [F([128, 1], F32) for _ in range(2)]
        P.dma("sp", lambda e: e.dma_start(out=g2B, in_=bc(ln2g_d, D)), writes=["g2B"])
        P.dma("sp", lambda e: e.dma_start(out=b2B, in_=bc(ln2b_d, D)), writes=["b2B"])

        def loads_d(tt):
            p2 = tt % 2
            P.dma("sp", lambda e: e.dma_start(out=ht[p2], in_=hrows_d[tt * 128:(tt + 1) * 128, :]), reads=["hrows_d"], writes=["ht%d" % p2])
            for k in range(4):
                P.dma("pool", lambda e, k=k: e.indirect_dma_start(
                    out=yk[k], out_offset=None, in_=yrows_d,
                    in_offset=bass.IndirectOffsetOnAxis(ap=idx_all[:, tt * 4 + k:tt * 4 + k + 1], axis=0)),
                    reads=["idx_all", "yrows_d"], writes=["yk%d" % k])

        loads_d(0)
        for tt in range(16):
            p2 = tt % 2
            P.act(lambda e, p2=p2: e.activation(out=acc[p2], in_=ht[p2], func=AF.Copy, scale=ALPHA), reads=["ht%d" % p2], writes=["L2src%d" % p2])
            for k in range(4):
                P.dve(lambda e, p2=p2, k=k, tt=tt: e.scalar_tensor_tensor(out=acc[p2], in0=yk[k], scalar=gates4[:, tt * 4 + k:tt * 4 + k + 1], in1=acc[p2],
                                                                          op0=ALU.mult, op1=ALU.add),
                      reads=["yk%d" % k, "gates4", "L2src%d" % p2], writes=["L2src%d" % p2])
            if tt + 1 < 16:
                loads_d(tt + 1)
            ln_rows(acc[p2], p2, st2[p2], mv2[p2], rs2[p2], "L2")
            P.dve(lambda e, p2=p2: e.scalar_tensor_tensor(out=acc[p2], in0=acc[p2], scalar=mv2[p2][:, 0:1], in1=g2B, op0=ALU.subtract, op1=ALU.mult),
                  reads=["L2src%d" % p2, "L2mv%d" % p2, "g2B"], writes=["L2src%d" % p2])
            P.dve(lambda e, p2=p2: e.scalar_tensor_tensor(out=acc[p2], in0=acc[p2], scalar=rs2[p2][:, 0:1], in1=b2B, op0=ALU.mult, op1=ALU.add),
                  reads=["L2src%d" % p2, "L2rs%d" % p2, "b2B"], writes=["L2src%d" % p2])
            P.dma("sp", lambda e, p2=p2, tt=tt: e.dma_start(out=out_d[tt * 128:(tt + 1) * 128, :], in_=acc[p2]),
                  reads=["L2src%d" % p2], writes=["out_d"], semkey=("st", "acc%d" % p2))
        P.add("sp", lambda e: e.nop(), reads=["out_d", "dbg"] if DEBUG else ["out_d"])
        P.emit(st)
    return nc


def _t5_bucket_np(dist):
    exact = 16
    d = np.maximum(dist, 1).astype(np.float32)
    large = exact + (np.log(d / np.float32(exact)) / np.float32(math.log(2048 / exact)) * np.float32(32 - exact)).astype(np.int32)
    large = np.minimum(large, 31)
    return np.where(dist < exact, dist, large)


def _bucket_tables():
    j = np.arange(128)[:, None]
    qi = np.arange(128)[None, :]
    cur_d = qi - j
    prev_d = qi + 128 - j
    return cur_d, prev_d


def prepare_shared(inp):
    f = np.float32
    sh = {}
    w_in = np.asarray(inp["w_in"])[0]
    Wr = w_in.reshape(16, 128, 15360)

    def blockify(cols):
        return np.ascontiguousarray(Wr[:, :, cols].transpose(1, 0, 2)).reshape(128, -1)

    sh["wz"] = np.stack([blockify(np.arange(O_Z + i * 512, O_Z + (i + 1) * 512)) for i in range(4)])
    wa = []
    for h in range(NH):
        for g in range(NG):
            base = g * 1024 + h * 128
            cols = np.concatenate([np.arange(base, base + 128), np.arange(O_K + base, O_K + base + 128),
                                   np.arange(O_V + base, O_V + base + 128)])
            wa.append(blockify(cols))
    sh["wa"] = np.stack(wa)
    sh["wg"] = np.stack([blockify(np.arange(O_G + i * 512, O_G + (i + 1) * 512)) for i in range(8)])
    rel = np.asarray(inp["rel_bias"])
    cur_d, prev_d = _bucket_tables()
    bm = np.empty((24, 128, 256), f)
    for h in range(NH):
        for g in range(NG):
            r = DIL[g]
            col = rel[:, g * NH + h]
            cur = col[_t5_bucket_np(r * np.clip(cur_d, 0, 128))]
            prv = col[_t5_bucket_np(r * np.clip(prev_d, 0, 128))]
            bm[h * 3 + g, :, 0:128] = np.where(cur_d >= 0, cur, f(NEG))
            bm[h * 3 + g, :, 128:256] = np.where(prev_d <= 128, prv, f(NEG))
    sh["bm"] = bm
    sw = np.asarray(inp["sgu_w"])[0]
    sh["swt"] = np.ascontiguousarray(sw.transpose(2, 0, 1)).reshape(128, 8 * 128)
    sh["tril"] = (np.arange(128)[:, None] <= np.arange(128)[None, :]).astype(f)
    sh["sgub"] = np.asarray(inp["sgu_b"])[0].reshape(1, 1024)
    sh["slng"] = np.asarray(inp["sgu_ln_g"]).reshape(1, 1024)
    sh["slnb"] = np.asarray(inp["sgu_ln_b"]).reshape(1, 1024)
    wb = np.asarray(inp["w_branch"])[0]
    sh["wbr"] = np.ascontiguousarray(wb.reshape(2, 8, 128, 16, 128).transpose(3, 2, 0, 1, 4)).reshape(16, 128, 2 * 8 * 128)
    wo = np.asarray(inp["w_out"])[0]
    sh["wout"] = np.ascontiguousarray(wo.reshape(16, 128, D).transpose(1, 0, 2)).reshape(128, 16 * D)
    sh["ln1g"] = np.asarray(inp["ln1_g"]).reshape(1, D)
    sh["ln1b"] = np.asarray(inp["ln1_b"]).reshape(1, D)
    wrt = np.asarray(inp["w_router"])[0]
    sh["wr"] = np.ascontiguousarray(wrt.reshape(16, 128, 32).transpose(1, 0, 2)).reshape(128, 16 * 32)
    sh["br"] = np.asarray(inp["b_router"]).reshape(1, 32)
    wgu = np.asarray(inp["w_gu"])[0]
    sh["wgu"] = np.ascontiguousarray(wgu.reshape(NE, 16, 128, 2, 8, 2, 128).transpose(0, 4, 2, 1, 5, 3, 6)).reshape(NE, 8, 128, 16 * 512)
    bgu = np.asarray(inp["b_gu"])[0]
    sh["bgu"] = np.ascontiguousarray(bgu.reshape(NE, 2, 16, 128).transpose(3, 0, 1, 2)).reshape(128, NE * 32)
    wd = np.asarray(inp["w_down"])[0]
    sh["wd"] = np.ascontiguousarray(wd.reshape(NE, 16, 128, 4, 512).transpose(0, 3, 2, 1, 4)).reshape(NE, 4, 128, 16 * 512)
    sh["bd"] = np.ascontiguousarray(np.asarray(inp["b_down"])[0])
    sh["ln2g"] = np.asarray(inp["ln2_g"]).reshape(1, D)
    sh["ln2b"] = np.asarray(inp["ln2_b"]).reshape(1, D)
    sh["ident"] = np.eye(128, dtype=f)
    sh["ustr"] = (np.arange(128)[:, None] < np.arange(128)[None, :]).astype(f)
    sh["ecrow"] = (np.arange(32) * CAP).astype(f).reshape(1, 32)
    sh["tokid"] = (np.arange(16)[None, :] * 128 + np.arange(128)[:, None]).astype(f)
    return {k: np.ascontiguousarray(v, dtype=f) for k, v in sh.items()}


def per_core_inputs(x_b, shared):
    m = dict(shared)
    xb = np.ascontiguousarray(x_b, dtype=np.float32)
    m["x"] = xb
    m["xT"] = np.ascontiguousarray(xb.T.reshape(16, 128, S).transpose(1, 0, 2)).reshape(128, 16 * S)
    return m


_NC_CACHE = {}


def kernel(**inputs):
    x = np.asarray(inputs["x"])
    shared = prepare_shared(inputs)
    if "nc" not in _NC_CACHE:
        _NC_CACHE["nc"] = build_program()
    nc = _NC_CACHE["nc"]
    in_maps = [per_core_inputs(x[b], shared) for b in range(NCORES)]
    res = run_bass_kernel_spmd(nc, in_maps, core_ids=list(range(NCORES)))
    out = np.stack([np.asarray(res.results[b]["out"]) for b in range(NCORES)], axis=0)
    return out.astype(np.float32)
```

```python
import math
from contextlib import ExitStack
import numpy as np
import concourse.bass as bass
import concourse.mybir as mybir
from concourse.bass_utils import run_bass_kernel_spmd

dt = mybir.dt
F32, BF16, I32 = dt.float32, dt.bfloat16, dt.int32
AF = mybir.ActivationFunctionType
ALU = mybir.AluOpType

SAME_ENGINE_SYNC = True
DEBUG = False
NCORES = 8

D = 2048
S = 2048
NH = 8
NG = 3
DIL = (1, 4, 16)
NE = 32
CAP = 384
NPRE = 0
NST = CAP // 128
ALPHA = 2.0 ** 0.25
EPS = 1e-5
SCALE = 128.0 ** -0.5
NEG = -30000.0
O_K, O_V, O_Z, O_G = 3072, 6144, 9216, 11264
ARENA = 204 * 1024


class _Op:
    __slots__ = ("eng", "fn", "deps", "dma", "semkey", "need_inc", "count")

    def __init__(self, eng, fn, deps, dma, semkey):
        self.eng = eng
        self.fn = fn
        self.deps = deps
        self.dma = dma
        self.semkey = semkey
        self.need_inc = False
        self.count = 0


class Prog:
    def __init__(self, nc):
        self.nc = nc
        self.ops = []
        self.res = {}
        self.pending_barrier = None

    def add(self, eng, fn, reads=(), writes=(), dma=False, semkey=None, nowaw=False):
        idx = len(self.ops)
        deps = set()
        for r in reads:
            st = self.res.get(r)
            if st is not None:
                deps.update(st[0])
        for w in writes:
            st = self.res.get(w)
            if st is None:
                continue
            if st[1]:
                deps.update(st[1])
                deps.update(st[0])
            elif not nowaw:
                deps.update(st[0])
        for r in reads:
            st = self.res.setdefault(r, [[], []])
            st[1].append(idx)
        for w in writes:
            st = self.res.setdefault(w, [[], []])
            others = [r for r in st[1] if r != idx]
            if others:
                st[0] = [idx]
                st[1] = []
            else:
                st[1] = []
                if nowaw:
                    st[0].append(idx)
                else:
                    st[0] = [idx]
        deps.discard(idx)
        if dma and semkey is None:
            semkey = ("dma", writes[0] if writes else idx)
        self.ops.append(_Op(eng, fn, deps, dma, semkey))
        return idx

    def pe(self, fn, reads=(), writes=(), **kw):
        return self.add("pe", fn, reads, writes, **kw)

    def act(self, fn, reads=(), writes=(), **kw):
        return self.add("act", fn, reads, writes, **kw)

    def dve(self, fn, reads=(), writes=(), **kw):
        return self.add("dve", fn, reads, writes, **kw)

    def pool(self, fn, reads=(), writes=(), **kw):
        return self.add("pool", fn, reads, writes, **kw)

    def dma(self, q, fn, reads=(), writes=(), semkey=None, nowaw=True):
        return self.add(q, fn, reads, writes, dma=True, semkey=semkey, nowaw=nowaw)

    def barrier(self, engines=("sp", "pe", "act", "dve", "pool")):
        deps = set()
        for st in self.res.values():
            deps.update(st[0])
            deps.update(st[1])
        idx = len(self.ops)
        op = _Op("sp", lambda e: e.nop(), deps, False, None)
        self.ops.append(op)
        self.res = {"__bar__": [[idx], []]}
        for en in engines:
            if en == "sp":
                continue
            self.add(en, lambda e: e.nop(), reads=["__bar__"])

    def emit(self, stack):
        nc = self.nc
        ops = self.ops
        for i, op in enumerate(ops):
            for d in op.deps:
                p = ops[d]
                if p.dma:
                    p.need_inc = True
                elif p.eng == op.eng and (p.eng == "pe" or not SAME_ENGINE_SYNC):
                    pass
                else:
                    p.need_inc = True
        sems = {}

        def get_sem(key):
            if key not in sems:
                sems[key] = stack.enter_context(nc.semaphore("s%d" % len(sems)))
            return sems[key]

        counters = {}
        for op in ops:
            if op.dma:
                op.need_inc = True
                k = op.semkey
                counters[k] = counters.get(k, 0) + 16
                op.count = counters[k]
                get_sem(k)
            elif op.need_inc:
                k = ("eng", op.eng)
                counters[k] = counters.get(k, 0) + 1
                op.count = counters[k]
                op.semkey = k
                get_sem(k)
        self.n_sems = len(sems)
        by_eng = {}
        for i, op in enumerate(ops):
            by_eng.setdefault(op.eng, []).append(i)
        block = stack.enter_context(nc.Block())
        deco = {"pe": block.tensor, "act": block.scalar, "dve": block.vector,
                "pool": block.gpsimd, "sp": block.sync}

        def make(engname):
            idxs = by_eng.get(engname, [])

            def body(eng):
                seen = {}
                for i in idxs:
                    op = ops[i]
                    need = {}
                    for d in op.deps:
                        p = ops[d]
                        if (not p.dma) and p.eng == engname and (engname == "pe" or not SAME_ENGINE_SYNC):
                            continue
                        k = p.semkey
                        if need.get(k, 0) < p.count:
                            need[k] = p.count
                    for k, v in need.items():
                        if seen.get(k, 0) < v:
                            eng.wait_ge(sems[k], v)
                            seen[k] = v
                    ins = op.fn(eng)
                    if op.need_inc:
                        ins.then_inc(sems[op.semkey], 16 if op.dma else 1)
            return body

        for engname in ["sp", "pe", "act", "dve", "pool"]:
            if engname in by_eng:
                deco[engname](make(engname))


def _dsize(d):
    return {F32: 4, BF16: 2, I32: 4}[d]


def build_program():
    nc = bass.Bass("TRN2", target_bir_lowering=False)
    scr_kind = "ExternalOutput" if DEBUG else "Internal"

    def din(name, shape, d=F32):
        return nc.dram_tensor(name, list(shape), d, kind="ExternalInput").ap()

    xT_d = din("xT", [128, 16 * S])
    x_d = din("x", [S, D])
    wz_d = din("wz", [4, 128, 16 * 512])
    wa_d = din("wa", [24, 128, 16 * 384])
    wg_d = din("wg", [8, 128, 16 * 512])
    bm_d = din("bm", [24, 128, 256])
    swt_d = din("swt", [128, 8 * 128])
    tril_d = din("tril", [128, 128])
    sgub_d = din("sgub", [1, 8 * 128])
    slng_d = din("slng", [1, 1024])
    slnb_d = din("slnb", [1, 1024])
    wbr_d = din("wbr", [16, 128, 2 * 8 * 128])
    wout_d = din("wout", [128, 16 * D])
    ln1g_d = din("ln1g", [1, D])
    ln1b_d = din("ln1b", [1, D])
    wr_d = din("wr", [128, 16 * 32])
    br_d = din("br", [1, 32])
    wgu_d = din("wgu", [NE, 8, 128, 16 * 512])
    bgu_d = din("bgu", [128, NE * 32])
    wd_d = din("wd", [NE, 4, 128, 16 * 512])
    bd_d = din("bd", [NE, D])
    ln2g_d = din("ln2g", [1, D])
    ln2b_d = din("ln2b", [1, D])
    ident_d = din("ident", [128, 128])
    ustr_d = din("ustr", [128, 128])
    ecrow_d = din("ecrow", [1, 32])
    tokid_d = din("tokid", [128, 16])

    out_d = nc.dram_tensor("out", [S, D], F32, kind="ExternalOutput").ap()
    ysgu_d = nc.dram_tensor("ysgu_s", [128, 8 * S], BF16, kind=scr_kind).ap()
    yattn_d = nc.dram_tensor("yattn_s", [8, 128, S], BF16, kind=scr_kind).ap()
    sg_d = nc.dram_tensor("sg_s", [16, 128, 2 * S], BF16, kind=scr_kind).ap()
    hrows_d = nc.dram_tensor("hrows_s", [S, D], F32, kind=scr_kind).ap()
    list_d = nc.dram_tensor("list_s", [NE * CAP + 128, 1], F32, kind=scr_kind).ap()
    yrows_d = nc.dram_tensor("yrows_s", [NE * CAP + 128, D], F32, kind=scr_kind).ap()
    wgu_bf = nc.dram_tensor("wgu_bf", [NE, 4, 128, 8192], BF16, kind="Internal").ap()
    dbg_d = nc.dram_tensor("dbg_s", [128, 16 * 32 + 64 + 64], F32, kind=scr_kind).ap()

    with ExitStack() as st:
        arena = st.enter_context(nc.sbuf_tensor("arena", [128, ARENA], dt.uint8))
        ps = [st.enter_context(nc.psum_tensor("ps%d" % i, [128, 512], F32)) for i in range(8)]
        P = Prog(nc)

        def V(off, shape, d):
            n = 1
            for s_ in shape[1:]:
                n *= s_
            nb = n * _dsize(d)
            assert off % 4 == 0 and off + nb <= ARENA, (off, nb)
            ap = arena[:, off:off + nb].bitcast(d)
            if len(shape) == 3:
                ap = ap.rearrange("p (a b) -> p a b", a=shape[1])
            elif len(shape) == 4:
                ap = ap.rearrange("p (a b c) -> p a b c", a=shape[1], b=shape[2])
            return ap

        class Alloc:
            def __init__(self, base):
                self.off = base

            def __call__(self, shape, d):
                n = 1
                for s_ in shape[1:]:
                    n *= s_
                nb = (n * _dsize(d) + 31) // 32 * 32
                v = V(self.off, shape, d)
                self.off += nb
                return v

        KB = 1024
        CA = Alloc(0)
        ident = CA([128, 128], F32)
        ustr = CA([128, 128], BF16)
        ones_bf = CA([128, 128], BF16)
        eps_t = CA([128, 1], F32)
        ecrow = CA([128, 32], F32)
        tokid = CA([128, 16], F32)
        brB = CA([128, 32], F32)
        logits = CA([128, 16, 32], F32)
        gates4 = CA([128, 64], F32)
        idx_all = CA([128, 64], I32)
        li = CA([128, 96], I32)
        carry = CA([128, 32], F32)
        assert CA.off <= 8 * KB, CA.off
        BASE = 8 * KB

        def bc(ap_row, n):
            return ap_row.broadcast_to([128, n])

        P.dma("sp", lambda e: e.dma_start(out=ident, in_=ident_d), writes=["ident"])
        P.dma("pool", lambda e: e.dma_start(out=ustr, in_=ustr_d), writes=["ustr"])
        P.dve(lambda e: e.memset(ones_bf, 1.0), writes=["ones_bf"])
        P.dve(lambda e: e.memset(eps_t, EPS), writes=["eps_t"])
        P.dma("sp", lambda e: e.dma_start(out=ecrow, in_=bc(ecrow_d, 32)), writes=["ecrow"])
        P.dma("sp", lambda e: e.dma_start(out=tokid, in_=tokid_d), writes=["tokid"])
        P.dma("sp", lambda e: e.dma_start(out=brB, in_=bc(br_d, 32)), writes=["brB"])

        bank_ctr = [0]
        ps_list = [("gu", e_, b_) for e_ in range(NE) for b_ in (1, 3, 5, 7)]
        ps_pos = [0]

        def prestage(n):
            for _ in range(n):
                if ps_pos[0] >= len(ps_list):
                    return
                kind, e_, b_ = ps_list[ps_pos[0]]
                ps_pos[0] += 1
                P.dma("pool", lambda e, e_=e_, b_=b_: e.dma_start(out=wgu_bf[e_, b_ // 2], in_=wgu_d[e_, b_], max_dma_last_dim=8192),
                      writes=["prestage"], semkey=("dma", "prestage"))

        def mm_group(psap, pairs, reads, psname):
            n = len(pairs)
            for i, (l, r) in enumerate(pairs):
                P.pe(lambda e, l=l, r=r, i=i: e.matmul(psap, lhsT=l, rhs=r, start=(i == 0), stop=(i == n - 1)),
                     reads=reads, writes=[psname])

        A = Alloc(BASE)
        xT = A([128, 16, S], BF16)
        ring = [A([128, 8192], BF16) for _ in range(2)]
        PH = A.off
        for kc in range(16):
            P.dma("pool", lambda e, kc=kc: e.dma_start(out=xT[:, kc, :], in_=xT_d[:, kc * S:(kc + 1) * S]),
                  writes=["xT"])

        G = Alloc(PH)
        uT = G([128, 8, S], BF16)
        vg = [G([128, 1024], F32) for _ in range(2)]
        vn = [G([128, 1024], F32) for _ in range(2)]
        vln = [G([128, 1024], BF16) for _ in range(2)]
        lngB = G([128, 1024], F32)
        lnbB = G([128, 1024], F32)
        bsB = G([128, 8, 128], F32)
        wsT = G([128, 8, 128], BF16)
        wsf = G([128, 8, 128], F32)
        trl = G([128, 128], F32)
        stats = [G([128, 4, 6], F32) for _ in range(2)]
        mv = [G([128, 2], F32) for _ in range(2)]
        rstd = [G([128, 1], F32) for _ in range(2)]
        mtmp = [G([128, 4, 128], F32) for _ in range(2)]

        P.dma("sp", lambda e: e.dma_start(out=lngB, in_=bc(slng_d, 1024)), writes=["lngB"])
        P.dma("sp", lambda e: e.dma_start(out=lnbB, in_=bc(slnb_d, 1024)), writes=["lnbB"])
        P.dma("sp", lambda e: e.dma_start(out=bsB.rearrange("p a b -> p (a b)"), in_=bc(sgub_d, 1024)), writes=["bsB"])
        P.dma("sp", lambda e: e.dma_start(out=wsf.rearrange("p a b -> p (a b)"), in_=swt_d), writes=["wsf"])
        P.dma("sp", lambda e: e.dma_start(out=trl, in_=tril_d), writes=["trl"])
        for g in range(8):
            P.dve(lambda e, g=g: e.tensor_tensor(out=wsT[:, g, :], in0=wsf[:, g, :], in1=trl, op=ALU.mult),
                  reads=["wsf", "trl"], writes=["wsT"], nowaw=True)

        def load_ring(slot, src, ncols):
            P.dma("pool", lambda e: e.dma_start(out=ring[slot][:, 0:16 * ncols], in_=src, max_dma_last_dim=8192),
                  writes=["ring%d" % slot])

        def ring3(slot, ncols):
            return ring[slot][:, 0:16 * ncols].rearrange("p (k c) -> p k c", k=16)

        load_ring(0, wz_d[0], 512)
        load_ring(1, wz_d[1], 512)
        for blk in range(2):
            W = ring3(blk, 512)
            for j in range(4):
                c = blk * 4 + j
                for tb in range(4):
                    b = bank_ctr[0] % 8
                    bank_ctr[0] += 1
                    mm_group(ps[b][:, :], [(W[:, kc, j * 128:(j + 1) * 128], xT[:, kc, tb * 512:(tb + 1) * 512]) for kc in range(16)],
                             ["ring%d" % blk, "xT"], "ps%d" % b)
                    P.act(lambda e, b=b, c=c, tb=tb: e.activation(out=uT[:, c, tb * 512:(tb + 1) * 512], in_=ps[b][:, :],
                                                                  func=AF.Gelu_apprx_tanh),
                          reads=["ps%d" % b], writes=["uT"], nowaw=True)
        load_ring(0, wz_d[2], 512)
        load_ring(1, wz_d[3], 512)
        def sgu_a(tt):
            p2 = tt % 2
            for vb in range(2):
                b = (tt % 2) * 2 + vb
                W = ring3(vb, 512)
                mm_group(ps[b][:, :], [(xT[:, kc, tt * 128:(tt + 1) * 128], W[:, kc, :]) for kc in range(16)],
                         ["ring%d" % vb, "xT"], "ps%d" % b)
                P.act(lambda e, b=b, vb=vb, p2=p2: e.activation(out=vg[p2][:, vb * 512:(vb + 1) * 512], in_=ps[b][:, :],
                                                                func=AF.Gelu_apprx_tanh),
                      reads=["ps%d" % b], writes=["vg%d" % p2], nowaw=True)
            for vb in range(2):
                P.dve(lambda e, vb=vb, p2=p2: e.bn_stats(out=stats[p2][:, vb, :], in_=vg[p2][:, vb * 512:(vb + 1) * 512]),
                      reads=["vg%d" % p2], writes=["stats%d" % p2], nowaw=True)
            P.dve(lambda e, p2=p2: e.bn_aggr(out=mv[p2], in_=stats[p2][:, 0:2, :]), reads=["stats%d" % p2], writes=["mv%d" % p2])
            P.act(lambda e, p2=p2: e.activation(out=rstd[p2], in_=mv[p2][:, 1:2], func=AF.Sqrt, bias=eps_t[:, 0:1], scale=1.0),
                  reads=["mv%d" % p2, "eps_t"], writes=["rstd%d" % p2])
            P.dve(lambda e, p2=p2: e.reciprocal(out=rstd[p2], in_=rstd[p2]), reads=["rstd%d" % p2], writes=["rstd%d" % p2])
            P.dve(lambda e, p2=p2: e.tensor_scalar(out=vn[p2], in0=vg[p2], scalar1=mv[p2][:, 0:1], scalar2=rstd[p2][:, 0:1],
                                                   op0=ALU.subtract, op1=ALU.mult),
                  reads=["vg%d" % p2, "mv%d" % p2, "rstd%d" % p2], writes=["vn%d" % p2])
            P.dve(lambda e, p2=p2: e.tensor_tensor(out=vn[p2], in0=vn[p2], in1=lngB, op=ALU.mult),
                  reads=["vn%d" % p2, "lngB"], writes=["vn%d" % p2])
            P.dve(lambda e, p2=p2: e.tensor_tensor(out=vln[p2], in0=vn[p2], in1=lnbB, op=ALU.add),
                  reads=["vn%d" % p2, "lnbB"], writes=["vln%d" % p2])

        def sgu_b(tt):
            p2 = tt % 2
            for half in range(2):
                b = 4 + (tt % 2) * 2 + half
                for gg in range(4):
                    g = half * 4 + gg
                    P.pe(lambda e, b=b, g=g, gg=gg, p2=p2: e.matmul(ps[b][:, gg * 128:(gg + 1) * 128], lhsT=vln[p2][:, g * 128:(g + 1) * 128],
                                                                    rhs=wsT[:, g, :], start=True, stop=True),
                         reads=["vln%d" % p2, "wsT"], writes=["ps%d" % b], nowaw=True)
                P.dve(lambda e, b=b, half=half, p2=p2: e.tensor_tensor(out=mtmp[p2], in0=ps[b][:, :].rearrange("p (a b) -> p a b", a=4),
                                                                       in1=bsB[:, half * 4:(half + 1) * 4, :], op=ALU.add),
                      reads=["ps%d" % b, "bsB"], writes=["mtmp%d" % p2])
                P.dve(lambda e, half=half, p2=p2, tt=tt: e.tensor_tensor(out=uT[:, half * 4:(half + 1) * 4, tt * 128:(tt + 1) * 128], in0=mtmp[p2],
                                                                        in1=uT[:, half * 4:(half + 1) * 4, tt * 128:(tt + 1) * 128], op=ALU.mult),
                      reads=["mtmp%d" % p2, "uT"], writes=["uT"], nowaw=True)
        for tt in range(16):
            sgu_a(tt)
            if tt >= 1:
                sgu_b(tt - 1)
        sgu_b(15)
        P.dma("sp", lambda e: e.dma_start(out=ysgu_d, in_=uT.rearrange("p a b -> p (a b)")), reads=["uT"], writes=["ysgu_d"])
        P.barrier()

        T = Alloc(PH + 32 * KB)
        qT = [T([128, S], BF16) for _ in range(2)]
        kT = [T([128, S], BF16) for _ in range(2)]
        vS = [T([128, 16, 128], BF16) for _ in range(2)]
        bmt = [T([128, 256], F32) for _ in range(2)]
        sbm = [T([128, 256], F32) for _ in range(3)]
        pT = [T([128, 256], BF16) for _ in range(3)]
        accN = T([128, S], F32)
        accD = T([128, S], F32)
        yout = [T([128, S], BF16) for _ in range(2)]
        sgt = [T([128, 512], BF16) for _ in range(4)]
        assert T.off <= ARENA, T.off

        units = [(h, g) for h in range(NH) for g in range(NG)]
        load_ring(0, wa_d[0], 384)
        sbi = [0]

        def gen_proj(ui):
            h, g = units[ui]
            r = DIL[g]
            L = S // r
            nb = L // 128
            slot = ui % 2
            par = ui % 2
            W = ring3(slot, 384)
            q3 = qT[par].rearrange("p (c m) -> p c m", c=r)
            k3 = kT[par].rearrange("p (c m) -> p c m", c=r)
            items = []

            def head():
                if ui + 1 < len(units):
                    load_ring((ui + 1) % 2, wa_d[ui + 1], 384)
                P.dma("sp", lambda e: e.dma_start(out=bmt[par], in_=bm_d[ui]), writes=["bmt%d" % par])
                prestage(3)
            items.append(head)
            for which, dst3 in ((0, q3), (1, k3)):
                for tb in range(4):
                    def grp(which=which, dst3=dst3, tb=tb):
                        b = tb % 2
                        mm_group(ps[b][:, :], [(W[:, kc, which * 128:(which + 1) * 128], xT[:, kc, tb * 512:(tb + 1) * 512]) for kc in range(16)],
                                 ["ring%d" % slot, "xT"], "ps%d" % b)
                        mw = 512 // r
                        dname = ("qT%d" if which == 0 else "kT%d") % par
                        P.act(lambda e: e.copy(out=dst3[:, :, tb * mw:(tb + 1) * mw], in_=ps[b][:, :].rearrange("p (m c) -> p c m", c=r)),
                              reads=["ps%d" % b], writes=[dname], nowaw=True)
                    items.append(grp)
            for tq in range(4):
                def grpv(tq=tq):
                    b = tq % 2
                    for t4 in range(4):
                        ti = tq * 4 + t4
                        c, n = ti // nb, ti % nb
                        t0 = c + r * 128 * n
                        for kc in range(16):
                            lhs = xT[:, kc, t0:t0 + r * 127 + 1:r]
                            P.pe(lambda e, t4=t4, lhs=lhs, kc=kc: e.matmul(ps[b][:, t4 * 128:(t4 + 1) * 128], lhsT=lhs, rhs=W[:, kc, 256:384],
                                                                       start=(kc == 0), stop=(kc == 15)),
                                 reads=["ring%d" % slot, "xT"], writes=["ps%d" % b], nowaw=True)
                    P.dve(lambda e: e.tensor_copy(out=vS[par][:, tq * 4:(tq + 1) * 4, :], in_=ps[b][:, :].rearrange("p (a b) -> p a b", a=4)),
                          reads=["ps%d" % b], writes=["vS%d" % par], nowaw=True)
                items.append(grpv)
            return items

        def gen_attn(ui):
            h, g = units[ui]
            r = DIL[g]
            L = S // r
            nb = L // 128
            par = ui % 2
            q3 = qT[par].rearrange("p (c m) -> p c m", c=r)
            k3 = kT[par].rearrange("p (c m) -> p c m", c=r)
            ob_started = set()
            cur_ob_box = [0]
            steps = [(c, kt, 256 if kt < nb - 1 else 128) for c in range(r) for kt in range(nb)]
            sb0 = sbi[0]
            sbi[0] += len(steps)

            def flush_bank(ob):
                nbk = 4 + ob % 2
                dbk = 6 + ob % 2
                if L >= 512:
                    c0 = (512 * ob) // L
                    m0 = (512 * ob) % L
                    vN = accN.rearrange("p (m c) -> p c m", c=r)[:, c0, m0:m0 + 512]
                    vD = accD.rearrange("p (m c) -> p c m", c=r)[:, c0, m0:m0 + 512]
                    pN = ps[nbk][:, :]
                    pD = ps[dbk][:, :]
                else:
                    nres = 512 // L
                    c0 = ob * nres
                    vN = accN.rearrange("p (m c) -> p c m", c=r)[:, c0:c0 + nres, :]
                    vD = accD.rearrange("p (m c) -> p c m", c=r)[:, c0:c0 + nres, :]
                    pN = ps[nbk][:, :].rearrange("p (c m) -> p c m", c=nres)
                    pD = ps[dbk][:, :].rearrange("p (c m) -> p c m", c=nres)
                if g == 0:
                    P.dve(lambda e: e.tensor_copy(out=vN, in_=pN), reads=["ps%d" % nbk], writes=["accN"], nowaw=True)
                    P.dve(lambda e: e.tensor_copy(out=vD, in_=pD), reads=["ps%d" % dbk], writes=["accD"], nowaw=True)
                else:
                    P.dve(lambda e: e.tensor_tensor(out=vN, in0=pN, in1=vN, op=ALU.add), reads=["ps%d" % nbk, "accN"], writes=["accN"], nowaw=True)
                    P.dve(lambda e: e.tensor_tensor(out=vD, in0=pD, in1=vD, op=ALU.add), reads=["ps%d" % dbk, "accD"], writes=["accD"], nowaw=True)

            def emit_S(i):
                c, kt, ncols = steps[i]
                sb_ = 2 + (sb0 + i) % 2
                si = (sb0 + i) % 3
                P.pe(lambda e: e.matmul(ps[sb_][:, 0:ncols], lhsT=k3[:, c, kt * 128:(kt + 1) * 128],
                                        rhs=q3[:, c, kt * 128:kt * 128 + ncols], start=True, stop=True),
                     reads=["kT%d" % par, "qT%d" % par], writes=["ps%d" % sb_])
                P.dve(lambda e: e.scalar_tensor_tensor(out=sbm[si][:, 0:ncols], in0=ps[sb_][:, 0:ncols], scalar=SCALE,
                                                       in1=bmt[par][:, 0:ncols], op0=ALU.mult, op1=ALU.add),
                      reads=["ps%d" % sb_, "bmt%d" % par], writes=["sbm%d" % si])
                P.act(lambda e: e.activation(out=pT[si][:, 0:ncols], in_=sbm[si][:, 0:ncols], func=AF.Exp),
                      reads=["sbm%d" % si], writes=["pT%d" % si])

            def emit_PV(i):
                c, kt, ncols = steps[i]
                si = (sb0 + i) % 3
                base = c * L + kt * 128
                if ncols == 256 and (base % 512) == 384:
                    pieces = [(base, 0, 128), (base + 128, 128, 128)]
                else:
                    pieces = [(base, 0, ncols)]
                for (lb, poff, pn) in pieces:
                    ob = lb // 512
                    if ob != cur_ob_box[0]:
                        flush_bank(cur_ob_box[0])
                        cur_ob_box[0] = ob
                    col = lb % 512
                    first = ob not in ob_started
                    ob_started.add(ob)
                    nbk = 4 + ob % 2
                    dbk = 6 + ob % 2
                    ti = c * nb + kt
                    P.pe(lambda e, nbk=nbk, col=col, pn=pn, ti=ti, poff=poff, first=first: e.matmul(
                        ps[nbk][:, col:col + pn], lhsT=vS[par][:, ti, :], rhs=pT[si][:, poff:poff + pn], start=first, stop=True),
                        reads=["vS%d" % par, "pT%d" % si], writes=["ps%d" % nbk], nowaw=True)
                    P.pe(lambda e, dbk=dbk, col=col, pn=pn, poff=poff, first=first: e.matmul(
                        ps[dbk][:, col:col + pn], lhsT=ones_bf, rhs=pT[si][:, poff:poff + pn], start=first, stop=True),
                        reads=["ones_bf", "pT%d" % si], writes=["ps%d" % dbk], nowaw=True)

            items = []
            n = len(steps)
            for i in range(n):
                def it(i=i):
                    emit_S(i)
                    if i >= 1:
                        emit_PV(i - 1)
                items.append(it)

            def tail():
                emit_PV(n - 1)
                flush_bank(cur_ob_box[0])
                if g == NG - 1:
                    yp = h % 2
                    P.dve(lambda e: e.reciprocal(out=accD, in_=accD), reads=["accD"], writes=["accD"])
                    P.dve(lambda e: e.tensor_tensor(out=yout[yp], in0=accN, in1=accD, op=ALU.mult),
                          reads=["accN", "accD"], writes=["yout%d" % yp])
                    P.dma("sp", lambda e: e.dma_start(out=yattn_d[h], in_=yout[yp]), reads=["yout%d" % yp], writes=["yattn_d"],
                          semkey=("st", "yout%d" % yp))
            items.append(tail)
            return items

        for it in gen_proj(0):
            it()
        for ui in range(len(units)):
            A_items = gen_attn(ui)
            P_items = gen_proj(ui + 1) if ui + 1 < len(units) else []
            na, npj = len(A_items), len(P_items)
            ai = 0
            for pi, pit in enumerate(P_items):
                pit()
                tgt = (pi + 1) * na // npj if npj else na
                while ai < tgt:
                    A_items[ai]()
                    ai += 1
            while ai < na:
                A_items[ai]()
                ai += 1

        gslot = len(units) % 2
        load_ring(gslot, wg_d[0], 512)
        sgi = 0
        for blk in range(8):
            slot = (gslot + blk) % 2
            if blk + 1 < 8:
                load_ring((slot + 1) % 2, wg_d[blk + 1], 512)
            prestage(2)
            W = ring3(slot, 512)
            for j in range(4):
                ci = blk * 4 + j
                n_, dc = ci // 16, ci % 16
                for tb in range(4):
                    b = bank_ctr[0] % 8
                    bank_ctr[0] += 1
                    mm_group(ps[b][:, :], [(W[:, kc, j * 128:(j + 1) * 128], xT[:, kc, tb * 512:(tb + 1) * 512]) for kc in range(16)],
                             ["ring%d" % slot, "xT"], "ps%d" % b)
                    s4 = sgi % 4
                    sgi += 1
                    P.act(lambda e, b=b, s4=s4: e.activation(out=sgt[s4], in_=ps[b][:, :], func=AF.Sigmoid),
                          reads=["ps%d" % b], writes=["sgt%d" % s4])
                    P.dma("sp", lambda e, s4=s4, dc=dc, n_=n_, tb=tb: e.dma_start(
                        out=sg_d[dc][:, n_ * S + tb * 512: n_ * S + (tb + 1) * 512], in_=sgt[s4]),
                        reads=["sgt%d" % s4], writes=["sg_d"], semkey=("st", "sgt%d" % s4))

        P.barrier()
        B = Alloc(BASE)
        gT = B([128, 16, S], BF16)
        yat = B([128, 8, S], BF16)
        ysg = B([128, 8, S], BF16)
        wbr = [B([128, 2, 8, 128], BF16) for _ in range(2)]
        sgl = [B([128, 2, S], BF16) for _ in range(2)]
        tg = [B([128, 512], F32) for _ in range(4)]
        PB2 = B.off
        P.dma("sp", lambda e: e.dma_start(out=yat, in_=yattn_d.rearrange("h p t -> p h t")), reads=["yattn_d"], writes=["yat"])
        P.dma("sp", lambda e: e.dma_start(out=ysg.rearrange("p a b -> p (a b)"), in_=ysgu_d), reads=["ysgu_d"], writes=["ysg"])

        def load_b1(dc):
            s2 = dc % 2
            P.dma("pool", lambda e: e.dma_start(out=wbr[s2].rearrange("p a b c -> p (a b c)"), in_=wbr_d[dc]), writes=["wbr%d" % s2])
            P.dma("sp", lambda e: e.dma_start(out=sgl[s2].rearrange("p a b -> p (a b)"), in_=sg_d[dc]), reads=["sg_d"], writes=["sgl%d" % s2])

        load_b1(0)
        tgi = 0
        for dc in range(16):
            s2 = dc % 2
            if dc + 1 < 16:
                load_b1(dc + 1)
            prestage(1)
            for tb in range(4):
                b0 = (bank_ctr[0] % 4) * 2
                bank_ctr[0] += 1
                b1 = b0 + 1
                mm_group(ps[b0][:, :], [(wbr[s2][:, 0, kc, :], yat[:, kc, tb * 512:(tb + 1) * 512]) for kc in range(8)],
                         ["wbr%d" % s2, "yat"], "ps%d" % b0)
                mm_group(ps[b1][:, :], [(wbr[s2][:, 1, kc, :], ysg[:, kc, tb * 512:(tb + 1) * 512]) for kc in range(8)],
                         ["wbr%d" % s2, "ysg"], "ps%d" % b1)
                ta, tb_ = tgi % 4, (tgi + 1) % 4
                tgi += 2
                P.dve(lambda e, b0=b0, ta=ta, s2=s2, tb=tb: e.tensor_tensor(out=tg[ta], in0=ps[b0][:, :], in1=sgl[s2][:, 0, tb * 512:(tb + 1) * 512], op=ALU.mult),
                      reads=["ps%d" % b0, "sgl%d" % s2], writes=["tg%d" % ta])
                P.dve(lambda e, b1=b1, tb_=tb_, s2=s2, tb=tb: e.tensor_tensor(out=tg[tb_], in0=ps[b1][:, :], in1=sgl[s2][:, 1, tb * 512:(tb + 1) * 512], op=ALU.mult),
                      reads=["ps%d" % b1, "sgl%d" % s2], writes=["tg%d" % tb_])
                P.dve(lambda e, ta=ta, tb_=tb_, dc=dc, tb=tb: e.tensor_tensor(out=gT[:, dc, tb * 512:(tb + 1) * 512], in0=tg[ta], in1=tg[tb_], op=ALU.add),
                      reads=["tg%d" % ta, "tg%d" % tb_], writes=["gT"], nowaw=True)

        P.barrier()
        C2 = Alloc(BASE + 64 * KB)
        wout = C2([128, 16, D], BF16)
        xt = [C2([128, D], F32) for _ in range(2)]
        pre = [C2([128, D], F32) for _ in range(2)]
        g1B = C2([128, D], F32)
        b1B = C2([128, D], F32)
        hTt = [C2([128, 16, 128], F32) for _ in range(2)]
        wr = C2([128, 16, 32], F32)
        st1 = [C2([128, 4, 6], F32) for _ in range(2)]
        mv1 = [C2([128, 2], F32) for _ in range(2)]
        rs1 = [C2([128, 1], F32) for _ in range(2)]
        top8 = [C2([128, 8], F32) for _ in range(2)]
        maskb = [C2([128, 32], BF16) for _ in range(2)]
        negm = [C2([128, 1], F32) for _ in range(2)]
        e4 = [C2([128, 4], F32) for _ in range(2)]
        esum = [C2([128, 1], F32) for _ in range(2)]
        posg = [C2([128, 32], F32) for _ in range(2)]
        junk = [C2([128, 32], F32) for _ in range(2)]
        idxf = [C2([128, 4], F32) for _ in range(2)]
        zl = C2([128, 97], F32)
        assert C2.off <= ARENA, C2.off

        for dc in range(16):
            P.dma("pool", lambda e, dc=dc: e.dma_start(out=wout[:, dc, :], in_=wout_d[:, dc * D:(dc + 1) * D]), writes=["wout"])
        P.dma("sp", lambda e: e.dma_start(out=g1B, in_=bc(ln1g_d, D)), writes=["g1B"])
        P.dma("sp", lambda e: e.dma_start(out=b1B, in_=bc(ln1b_d, D)), writes=["b1B"])
        P.dma("sp", lambda e: e.dma_start(out=wr.rearrange("p a b -> p (a b)"), in_=wr_d), writes=["wr"])
        P.dve(lambda e: e.memset(zl, 0.0), writes=["zl"])
        P.dma("sp", lambda e: e.dma_start(out=list_d.rearrange("(p r) o -> p (r o)", p=128), in_=zl), reads=["zl"], writes=["list_d"])
        P.dve(lambda e: e.tensor_copy(out=carry, in_=ecrow), reads=["ecrow"], writes=["carry"])

        def ln_rows(src, p2, stt, mvv, rss, tagp):
            for cb in range(4):
                P.dve(lambda e, cb=cb: e.bn_stats(out=stt[:, cb, :], in_=src[:, cb * 512:(cb + 1) * 512]),
                      reads=[tagp + "src%d" % p2], writes=[tagp + "st%d" % p2], nowaw=True)
            P.dve(lambda e: e.bn_aggr(out=mvv, in_=stt), reads=[tagp + "st%d" % p2], writes=[tagp + "mv%d" % p2])
            P.act(lambda e: e.activation(out=rss, in_=mvv[:, 1:2], func=AF.Sqrt, bias=eps_t[:, 0:1], scale=1.0),
                  reads=[tagp + "mv%d" % p2, "eps_t"], writes=[tagp + "rs%d" % p2])
            P.dve(lambda e: e.reciprocal(out=rss, in_=rss), reads=[tagp + "rs%d" % p2], writes=[tagp + "rs%d" % p2])

        P.dma("sp", lambda e: e.dma_start(out=xt[0], in_=x_d[0:128, :]), writes=["xt0"])
        def b2_part1(tt):
            prestage(2)
            p2 = tt % 2
            if tt + 1 < 16:
                P.dma("sp", lambda e, tt=tt: e.dma_start(out=xt[(tt + 1) % 2], in_=x_d[(tt + 1) * 128:(tt + 2) * 128, :]),
                      writes=["xt%d" % ((tt + 1) % 2)])
            for cb in range(4):
                b = cb
                mm_group(ps[b][:, :], [(gT[:, dc, tt * 128:(tt + 1) * 128], wout[:, dc, cb * 512:(cb + 1) * 512]) for dc in range(16)],
                         ["gT", "wout"], "ps%d" % b)
                P.dve(lambda e, b=b, cb=cb, p2=p2: e.scalar_tensor_tensor(out=pre[p2][:, cb * 512:(cb + 1) * 512], in0=xt[p2][:, cb * 512:(cb + 1) * 512],
                                                                          scalar=ALPHA, in1=ps[b][:, :], op0=ALU.mult, op1=ALU.add),
                      reads=["ps%d" % b, "xt%d" % p2], writes=["L1src%d" % p2], nowaw=True)
            ln_rows(pre[p2], p2, st1[p2], mv1[p2], rs1[p2], "L1")
            P.dve(lambda e, p2=p2: e.scalar_tensor_tensor(out=pre[p2], in0=pre[p2], scalar=mv1[p2][:, 0:1], in1=g1B, op0=ALU.subtract, op1=ALU.mult),
                  reads=["L1src%d" % p2, "L1mv%d" % p2, "g1B"], writes=["L1src%d" % p2])
            P.dve(lambda e, p2=p2: e.scalar_tensor_tensor(out=pre[p2], in0=pre[p2], scalar=rs1[p2][:, 0:1], in1=b1B, op0=ALU.mult, op1=ALU.add),
                  reads=["L1src%d" % p2, "L1rs%d" % p2, "b1B"], writes=["L1src%d" % p2])
            P.dma("sp", lambda e, p2=p2, tt=tt: e.dma_start(out=hrows_d[tt * 128:(tt + 1) * 128, :], in_=pre[p2]),
                  reads=["L1src%d" % p2], writes=["hrows_d"], semkey=("st", "pre%d" % p2))

        def b2_part2(tt):
            p2 = tt % 2
            for q4 in range(4):
                b = 4 + q4 % 2
                for j in range(4):
                    kc = q4 * 4 + j
                    P.pe(lambda e, b=b, j=j, kc=kc, p2=p2: e.transpose(out=ps[b][:, j * 128:(j + 1) * 128], in_=pre[p2][:, kc * 128:(kc + 1) * 128], identity=ident),
                         reads=["L1src%d" % p2, "ident"], writes=["ps%d" % b], nowaw=True)
                P.act(lambda e, b=b, q4=q4, p2=p2: e.copy(out=hTt[p2][:, q4 * 4:(q4 + 1) * 4, :], in_=ps[b][:, :].rearrange("p (a b) -> p a b", a=4)),
                      reads=["ps%d" % b], writes=["hT%d" % p2], nowaw=True)
            mm_group(ps[6][:, 0:32], [(hTt[p2][:, kc, :], wr[:, kc, :]) for kc in range(16)], ["hT%d" % p2, "wr"], "ps6")
            P.dve(lambda e, tt=tt: e.tensor_tensor(out=logits[:, tt, :], in0=ps[6][:, 0:32], in1=brB, op=ALU.add),
                  reads=["ps6", "brB"], writes=["lg%d" % p2])

        def b3_route(tt):
            p2 = tt % 2
            lg = logits[:, tt, :]
            P.dve(lambda e, p2=p2, lg=lg: e.max(out=top8[p2], in_=lg), reads=["lg%d" % p2], writes=["top8%d" % p2])
            P.dve(lambda e, p2=p2, lg=lg: e.tensor_scalar(out=maskb[p2], in0=lg, scalar1=top8[p2][:, 3:4], scalar2=None, op0=ALU.is_ge),
                  reads=["lg%d" % p2, "top8%d" % p2], writes=["maskb%d" % p2])
            P.dve(lambda e, p2=p2: e.tensor_scalar(out=negm[p2], in0=top8[p2][:, 0:1], scalar1=-1.0, scalar2=None, op0=ALU.mult),
                  reads=["top8%d" % p2], writes=["negm%d" % p2])
            P.act(lambda e, p2=p2: e.activation(out=e4[p2], in_=top8[p2][:, 0:4], func=AF.Exp, bias=negm[p2][:, 0:1], scale=1.0),
                  reads=["top8%d" % p2, "negm%d" % p2], writes=["e4%d" % p2])
            P.dve(lambda e, p2=p2: e.reduce_sum(out=esum[p2], in_=e4[p2], axis=mybir.AxisListType.X),
                  reads=["e4%d" % p2], writes=["esum%d" % p2])
            P.dve(lambda e, p2=p2: e.reciprocal(out=esum[p2], in_=esum[p2]), reads=["esum%d" % p2], writes=["esum%d" % p2])
            P.dve(lambda e, p2=p2, tt=tt: e.tensor_scalar(out=gates4[:, tt * 4:(tt + 1) * 4], in0=e4[p2], scalar1=esum[p2][:, 0:1], scalar2=None, op0=ALU.mult),
                  reads=["e4%d" % p2, "esum%d" % p2], writes=["gates4"], nowaw=True)
            P.pe(lambda e, p2=p2: e.matmul(ps[7][:, 0:32], lhsT=ustr, rhs=maskb[p2], start=True, stop=True),
                 reads=["ustr", "maskb%d" % p2], writes=["ps7a"])
            P.pe(lambda e, p2=p2: e.matmul(ps[7][:, 32:64], lhsT=ones_bf, rhs=maskb[p2], start=True, stop=True),
                 reads=["ones_bf", "maskb%d" % p2], writes=["ps7b"])
            P.dve(lambda e, p2=p2: e.tensor_tensor(out=posg[p2], in0=ps[7][:, 0:32], in1=carry, op=ALU.add),
                  reads=["ps7a", "carry"], writes=["posg%d" % p2])
            P.dve(lambda e: e.tensor_tensor(out=carry, in0=ps[7][:, 32:64], in1=carry, op=ALU.add),
                  reads=["ps7b", "carry"], writes=["carry"])
            for k in range(4):
                P.dve(lambda e, p2=p2, k=k, lg=lg: e.scalar_tensor_tensor(out=junk[p2], in0=lg, scalar=top8[p2][:, k:k + 1], in1=posg[p2],
                                                                          op0=ALU.is_equal, op1=ALU.mult),
                      reads=["lg%d" % p2, "top8%d" % p2, "posg%d" % p2], writes=["junk%d" % p2])
                P.dve(lambda e, p2=p2, k=k: e.reduce_sum(out=idxf[p2][:, k:k + 1], in_=junk[p2], axis=mybir.AxisListType.X),
                      reads=["junk%d" % p2], writes=["idxf%d" % p2], nowaw=True)
            P.dve(lambda e, p2=p2, tt=tt: e.tensor_scalar(out=idx_all[:, tt * 4:(tt + 1) * 4], in0=idxf[p2], scalar1=0.0, scalar2=float(NE * CAP + 127),
                                                          op0=ALU.max, op1=ALU.min),
                  reads=["idxf%d" % p2], writes=["idx_all"], nowaw=True)
            for k in range(4):
                P.dma("pool", lambda e, tt=tt, k=k: e.indirect_dma_start(
                    out=list_d, out_offset=bass.IndirectOffsetOnAxis(ap=idx_all[:, tt * 4 + k:tt * 4 + k + 1], axis=0),
                    in_=tokid[:, tt:tt + 1], in_offset=None),
                    reads=["idx_all", "tokid", "list_d"], writes=["list_d2"])
        for tt in range(16):
            b2_part1(tt)
            if tt >= 1:
                b2_part2(tt - 1)
        b2_part2(15)
        for tt in range(16):
            b3_route(tt)
        if DEBUG:
            P.dma("sp", lambda e: e.dma_start(out=dbg_d[:, 0:512], in_=logits.rearrange("p a b -> p (a b)")), reads=["lg0", "lg1"], writes=["dbg"])
            P.dma("sp", lambda e: e.dma_start(out=dbg_d[:, 512:576], in_=gates4), reads=["gates4"], writes=["dbg"])
            P.dma("sp", lambda e: e.dma_start(out=dbg_d[:, 576:640], in_=idx_all.bitcast(F32)), reads=["idx_all"], writes=["dbg"])

        P.barrier()
        M = Alloc(BASE)
        lf = M([128, 128], F32)
        bgu = M([128, NE, 32], F32)
        Xe = [M([128, D], F32) for _ in range(3)]
        XeT = [M([128, 16, CAP], BF16) for _ in range(2)]
        wgu = [M([128, 8192], BF16) for _ in range(3)]
        wdn = [M([128, 8192], BF16) for _ in range(2)]
        actT = [M([128, 16, CAP], BF16) for _ in range(2)]
        gl = [M([128, CAP], F32) for _ in range(2)]
        sgm = [M([128, CAP], F32) for _ in range(2)]
        ln_ = [M([128, CAP], F32) for _ in range(2)]
        ypc = [M([128, 512], F32) for _ in range(4)]
        bdB = [M([128, D], F32) for _ in range(2)]
        assert M.off <= ARENA, M.off

        P.dma("sp", lambda e: e.dma_start(out=lf[0:96, :], in_=list_d[0:NE * CAP, :].rearrange("(n p) o -> n (p o)", p=128)),
              reads=["list_d", "list_d2"], writes=["lf"])
        P.dma("sp", lambda e: e.dma_start(out=bgu.rearrange("p a b -> p (a b)"), in_=bgu_d), writes=["bgu"])
        P.pe(lambda e: e.transpose(out=ps[0][:, 0:96], in_=lf[0:96, :], identity=ident[0:96, 0:96]), reads=["lf", "ident"], writes=["ps0"])
        P.dve(lambda e: e.tensor_scalar(out=li, in0=ps[0][:, 0:96], scalar1=0.0, scalar2=float(S - 1), op0=ALU.max, op1=ALU.min),
              reads=["ps0"], writes=["li"])

        wstream = []
        for e_ in range(NE):
            for blk in range(8):
                wstream.append(("gu", e_, blk))
            for db in range(4):
                wstream.append(("dn", e_, db))
        cnt = {"gu": 0, "dn": 0}
        slot_of = {}
        issued = [0]

        consumed = {"gu": 0, "dn": 0}
        capk = {"gu": 3, "dn": 2}

        def pump():
            while issued[0] < len(wstream):
                kind, e_, bi = wstream[issued[0]]
                if cnt[kind] - consumed[kind] >= capk[kind]:
                    break
                if kind == "gu":
                    s_ = cnt["gu"] % 3
                    if bi % 2 == 1:
                        P.dma("sp", lambda e, s_=s_, e_=e_, bi=bi: e.dma_start(out=wgu[s_], in_=wgu_bf[e_, bi // 2]), writes=["wgu%d" % s_])
                    else:
                        P.dma("pool", lambda e, s_=s_, e_=e_, bi=bi: e.dma_start(out=wgu[s_], in_=wgu_d[e_, bi], max_dma_last_dim=8192), writes=["wgu%d" % s_])
                else:
                    s_ = cnt["dn"] % 2
                    P.dma("pool", lambda e, s_=s_, e_=e_, bi=bi: e.dma_start(out=wdn[s_], in_=wd_d[e_, bi], max_dma_last_dim=8192), writes=["wdn%d" % s_])
                cnt[kind] += 1
                slot_of[issued[0]] = s_
                issued[0] += 1

        def gather_x(e_):
            for s3 in range(NST):
                P.dma("pool", lambda e, e_=e_, s3=s3: e.indirect_dma_start(
                    out=Xe[s3], out_offset=None, in_=hrows_d,
                    in_offset=bass.IndirectOffsetOnAxis(ap=li[:, e_ * NST + s3:e_ * NST + s3 + 1], axis=0)),
                    reads=["li", "hrows_d"], writes=["Xe%d" % s3])

        gather_x(0)
        wi = 0
        tci = 0
        yi = 0
        for e_ in range(NE):
            xp = e_ % 2
            P.dma("sp", lambda e, e_=e_, xp=xp: e.dma_start(out=bdB[xp], in_=bc(bd_d[e_:e_ + 1, :], D)), writes=["bdB%d" % xp])
            for s3 in range(NST):
                for q4 in range(4):
                    b = tci % 2
                    tci += 1
                    for j in range(4):
                        kc = q4 * 4 + j
                        P.pe(lambda e, b=b, j=j, kc=kc, s3=s3: e.transpose(out=ps[b][:, j * 128:(j + 1) * 128], in_=Xe[s3][:, kc * 128:(kc + 1) * 128], identity=ident),
                             reads=["Xe%d" % s3, "ident"], writes=["ps%d" % b], nowaw=True)
                    P.act(lambda e, b=b, q4=q4, s3=s3, xp=xp: e.copy(out=XeT[xp][:, q4 * 4:(q4 + 1) * 4, s3 * 128:(s3 + 1) * 128],
                                                                     in_=ps[b][:, :].rearrange("p (a b) -> p a b", a=4)),
                          reads=["ps%d" % b], writes=["XeT%d" % xp], nowaw=True)
            if e_ + 1 < NE:
                gather_x(e_ + 1)
            for blk in range(8):
                pump()
                s_ = slot_of[wi]
                wi += 1
                W = wgu[s_].rearrange("p (k c) -> p k c", k=16)
                for mm_ in range(2):
                    m = blk * 2 + mm_
                    pg = 2 + (m % 2) * 2
                    pl = pg + 1
                    a2 = m % 2
                    mm_group(ps[pg][:, 0:CAP], [(W[:, kc, mm_ * 256:mm_ * 256 + 128], XeT[xp][:, kc, :]) for kc in range(16)],
                             ["wgu%d" % s_, "XeT%d" % xp], "ps%d" % pg)
                    mm_group(ps[pl][:, 0:CAP], [(W[:, kc, mm_ * 256 + 128:mm_ * 256 + 256], XeT[xp][:, kc, :]) for kc in range(16)],
                             ["wgu%d" % s_, "XeT%d" % xp], "ps%d" % pl)
                    P.dve(lambda e, pg=pg, a2=a2, e_=e_, m=m: e.tensor_scalar(out=gl[a2], in0=ps[pg][:, 0:CAP], scalar1=bgu[:, e_, m:m + 1], scalar2=7.0,
                                                                              op0=ALU.add, op1=ALU.min),
                          reads=["ps%d" % pg, "bgu"], writes=["gl%d" % a2])
                    P.act(lambda e, a2=a2: e.activation(out=sgm[a2], in_=gl[a2], func=AF.Sigmoid, scale=1.702),
                          reads=["gl%d" % a2], writes=["sgm%d" % a2])
                    P.dve(lambda e, pl=pl, a2=a2, e_=e_, m=m: e.tensor_scalar(out=ln_[a2], in0=ps[pl][:, 0:CAP], scalar1=bgu[:, e_, 16 + m:17 + m], scalar2=7.0,
                                                                              op0=ALU.add, op1=ALU.min),
                          reads=["ps%d" % pl, "bgu"], writes=["ln%d" % a2])
                    P.dve(lambda e, a2=a2: e.tensor_scalar(out=ln_[a2], in0=ln_[a2], scalar1=-7.0, scalar2=1.0, op0=ALU.max, op1=ALU.add),
                          reads=["ln%d" % a2], writes=["ln%d" % a2])
                    P.dve(lambda e, a2=a2: e.tensor_tensor(out=gl[a2], in0=gl[a2], in1=sgm[a2], op=ALU.mult),
                          reads=["gl%d" % a2, "sgm%d" % a2], writes=["gl%d" % a2])
                    P.dve(lambda e, a2=a2, xp=xp, m=m: e.tensor_tensor(out=actT[xp][:, m, :], in0=gl[a2], in1=ln_[a2], op=ALU.mult),
                          reads=["gl%d" % a2, "ln%d" % a2], writes=["actT%d" % xp], nowaw=True)
                consumed["gu"] += 1
                pump()
            for db in range(4):
                pump()
                s_ = slot_of[wi]
                wi += 1
                Wd = wdn[s_].rearrange("p (k c) -> p k c", k=16)
                for s3 in range(NST):
                    b = 6 + yi % 2
                    y4 = yi % 4
                    yi += 1
                    mm_group(ps[b][:, :], [(actT[xp][:, hc, s3 * 128:(s3 + 1) * 128], Wd[:, hc, :]) for hc in range(16)],
                             ["wdn%d" % s_, "actT%d" % xp], "ps%d" % b)
                    P.dve(lambda e, b=b, y4=y4, xp=xp, db=db: e.tensor_tensor(out=ypc[y4], in0=ps[b][:, :], in1=bdB[xp][:, db * 512:(db + 1) * 512], op=ALU.add),
                          reads=["ps%d" % b, "bdB%d" % xp], writes=["ypc%d" % y4])
                    r0 = e_ * CAP + s3 * 128
                    P.dma("sp", lambda e, y4=y4, r0=r0, db=db: e.dma_start(out=yrows_d[r0:r0 + 128, db * 512:(db + 1) * 512], in_=ypc[y4]),
                          reads=["ypc%d" % y4], writes=["yrows_d"], semkey=("st", "ypc%d" % y4))
                consumed["dn"] += 1
                pump()

        P.barrier()
        F = Alloc(BASE)
        ht = [F([128, D], F32) for _ in range(2)]
        yk = [F([128, D], F32) for _ in range(4)]
        acc = [F([128, D], F32) for _ in range(2)]
        g2B = F([128, D], F32)
        b2B = F([128, D], F32)
        st2 = [F([128, 4, 6], F32) for _ in range(2)]
        mv2 = [F([128, 2], F32) for _ in range(2)]
        rs2 = [F([128, 1], F32) for _ in range(2)]
        P.dma("sp", lambda e: e.dma_start(out=g2B, in_=bc(ln2g_d, D)), writes=["g2B"])
        P.dma("sp", lambda e: e.dma_start(out=b2B, in_=bc(ln2b_d, D)), writes=["b2B"])

        def loads_d(tt):
            p2 = tt % 2
            P.dma("sp", lambda e: e.dma_start(out=ht[p2], in_=hrows_d[tt * 128:(tt + 1) * 128, :]), reads=["hrows_d"], writes=["ht%d" % p2])
            for k in range(4):
                P.dma("pool", lambda e, k=k: e.indirect_dma_start(
                    out=yk[k], out_offset=None, in_=yrows_d,
                    in_offset=bass.IndirectOffsetOnAxis(ap=idx_all[:, tt * 4 + k:tt * 4 + k + 1], axis=0)),
                    reads=["idx_all", "yrows_d"], writes=["yk%d" % k])

        loads_d(0)
        for tt in range(16):
            p2 = tt % 2
            P.act(lambda e, p2=p2: e.activation(out=acc[p2], in_=ht[p2], func=AF.Copy, scale=ALPHA), reads=["ht%d" % p2], writes=["L2src%d" % p2])
            for k in range(4):
                P.dve(lambda e, p2=p2, k=k, tt=tt: e.scalar_tensor_tensor(out=acc[p2], in0=yk[k], scalar=gates4[:, tt * 4 + k:tt * 4 + k + 1], in1=acc[p2],
                                                                          op0=ALU.mult, op1=ALU.add),
                      reads=["yk%d" % k, "gates4", "L2src%d" % p2], writes=["L2src%d" % p2])
            if tt + 1 < 16:
                loads_d(tt + 1)
            ln_rows(acc[p2], p2, st2[p2], mv2[p2], rs2[p2], "L2")
            P.dve(lambda e, p2=p2: e.scalar_tensor_tensor(out=acc[p2], in0=acc[p2], scalar=mv2[p2][:, 0:1], in1=g2B, op0=ALU.subtract, op1=ALU.mult),
                  reads=["L2src%d" % p2, "L2mv%d" % p2, "g2B"], writes=["L2src%d" % p2])
            P.dve(lambda e, p2=p2: e.scalar_tensor_tensor(out=acc[p2], in0=acc[p2], scalar=rs2[p2][:, 0:1], in1=b2B, op0=ALU.mult, op1=ALU.add),
                  reads=["L2src%d" % p2, "L2rs%d" % p2, "b2B"], writes=["L2src%d" % p2])
            P.dma("sp", lambda e, p2=p2, tt=tt: e.dma_start(out=out_d[tt * 128:(tt + 1) * 128, :], in_=acc[p2]),
                  reads=["L2src%d" % p2], writes=["out_d"], semkey=("st", "acc%d" % p2))
        P.add("sp", lambda e: e.nop(), reads=["out_d", "dbg"] if DEBUG else ["out_d"])
        P.emit(st)
    return nc


def _t5_bucket_np(dist):
    exact = 16
    d = np.maximum(dist, 1).astype(np.float32)
    large = exact + (np.log(d / np.float32(exact)) / np.float32(math.log(2048 / exact)) * np.float32(32 - exact)).astype(np.int32)
    large = np.minimum(large, 31)
    return np.where(dist < exact, dist, large)


def _bucket_tables():
    j = np.arange(128)[:, None]
    qi = np.arange(128)[None, :]
    cur_d = qi - j
    prev_d = qi + 128 - j
    return cur_d, prev_d


def prepare_shared(inp):
    f = np.float32
    sh = {}
    w_in = np.asarray(inp["w_in"])[0]
    Wr = w_in.reshape(16, 128, 15360)

    def blockify(cols):
        return np.ascontiguousarray(Wr[:, :, cols].transpose(1, 0, 2)).reshape(128, -1)

    sh["wz"] = np.stack([blockify(np.arange(O_Z + i * 512, O_Z + (i + 1) * 512)) for i in range(4)])
    wa = []
    for h in range(NH):
        for g in range(NG):
            base = g * 1024 + h * 128
            cols = np.concatenate([np.arange(base, base + 128), np.arange(O_K + base, O_K + base + 128),
                                   np.arange(O_V + base, O_V + base + 128)])
            wa.append(blockify(cols))
    sh["wa"] = np.stack(wa)
    sh["wg"] = np.stack([blockify(np.arange(O_G + i * 512, O_G + (i + 1) * 512)) for i in range(8)])
    rel = np.asarray(inp["rel_bias"])
    cur_d, prev_d = _bucket_tables()
    bm = np.empty((24, 128, 256), f)
    for h in range(NH):
        for g in range(NG):
            r = DIL[g]
            col = rel[:, g * NH + h]
            cur = col[_t5_bucket_np(r * np.clip(cur_d, 0, 128))]
            prv = col[_t5_bucket_np(r * np.clip(prev_d, 0, 128))]
            bm[h * 3 + g, :, 0:128] = np.where(cur_d >= 0, cur, f(NEG))
            bm[h * 3 + g, :, 128:256] = np.where(prev_d <= 128, prv, f(NEG))
    sh["bm"] = bm
    sw = np.asarray(inp["sgu_w"])[0]
    sh["swt"] = np.ascontiguousarray(sw.transpose(2, 0, 1)).reshape(128, 8 * 128)
    sh["tril"] = (np.arange(128)[:, None] <= np.arange(128)[None, :]).astype(f)
    sh["sgub"] = np.asarray(inp["sgu_b"])[0].reshape(1, 1024)
    sh["slng"] = np.asarray(inp["sgu_ln_g"]).reshape(1, 1024)
    sh["slnb"] = np.asarray(inp["sgu_ln_b"]).reshape(1, 1024)
    wb = np.asarray(inp["w_branch"])[0]
    sh["wbr"] = np.ascontiguousarray(wb.reshape(2, 8, 128, 16, 128).transpose(3, 2, 0, 1, 4)).reshape(16, 128, 2 * 8 * 128)
    wo = np.asarray(inp["w_out"])[0]
    sh["wout"] = np.ascontiguousarray(wo.reshape(16, 128, D).transpose(1, 0, 2)).reshape(128, 16 * D)
    sh["ln1g"] = np.asarray(inp["ln1_g"]).reshape(1, D)
    sh["ln1b"] = np.asarray(inp["ln1_b"]).reshape(1, D)
    wrt = np.asarray(inp["w_router"])[0]
    sh["wr"] = np.ascontiguousarray(wrt.reshape(16, 128, 32).transpose(1, 0, 2)).reshape(128, 16 * 32)
    sh["br"] = np.asarray(inp["b_router"]).reshape(1, 32)
    wgu = np.asarray(inp["w_gu"])[0]
    sh["wgu"] = np.ascontiguousarray(wgu.reshape(NE, 16, 128, 2, 8, 2, 128).transpose(0, 4, 2, 1, 5, 3, 6)).reshape(NE, 8, 128, 16 * 512)
    bgu = np.asarray(inp["b_gu"])[0]
    sh["bgu"] = np.ascontiguousarray(bgu.reshape(NE, 2, 16, 128).transpose(3, 0, 1, 2)).reshape(128, NE * 32)
    wd = np.asarray(inp["w_down"])[0]
    sh["wd"] = np.ascontiguousarray(wd.reshape(NE, 16, 128, 4, 512).transpose(0, 3, 2, 1, 4)).reshape(NE, 4, 128, 16 * 512)
    sh["bd"] = np.ascontiguousarray(np.asarray(inp["b_down"])[0])
    sh["ln2g"] = np.asarray(inp["ln2_g"]).reshape(1, D)
    sh["ln2b"] = np.asarray(inp["ln2_b"]).reshape(1, D)
    sh["ident"] = np.eye(128, dtype=f)
    sh["ustr"] = (np.arange(128)[:, None] < np.arange(128)[None, :]).astype(f)
    sh["ecrow"] = (np.arange(32) * CAP).astype(f).reshape(1, 32)
    sh["tokid"] = (np.arange(16)[None, :] * 128 + np.arange(128)[:, None]).astype(f)
    return {k: np.ascontiguousarray(v, dtype=f) for k, v in sh.items()}


def per_core_inputs(x_b, shared):
    m = dict(shared)
    xb = np.ascontiguousarray(x_b, dtype=np.float32)
    m["x"] = xb
    m["xT"] = np.ascontiguousarray(xb.T.reshape(16, 128, S).transpose(1, 0, 2)).reshape(128, 16 * S)
    return m


_NC_CACHE = {}


def kernel(**inputs):
    x = np.asarray(inputs["x"])
    shared = prepare_shared(inputs)
    if "nc" not in _NC_CACHE:
        _NC_CACHE["nc"] = build_program()
    nc = _NC_CACHE["nc"]
    in_maps = [per_core_inputs(x[b], shared) for b in range(NCORES)]
    res = run_bass_kernel_spmd(nc, in_maps, core_ids=list(range(NCORES)))
    out = np.stack([np.asarray(res.results[b]["out"]) for b in range(NCORES)], axis=0)
    return out.astype(np.float32)
```
